# Optimizing a Trainium2 kernel written in Bass

```python
import jax, jax.numpy as jnp
from jax import lax
import numpy as np

D_MODEL = 4096
BATCH = 1
SEQ = 8192
DEPTH = 1

ATTN_HEADS = 16
HEAD_DIM = 128
ATTN_WIDTH = ATTN_HEADS * HEAD_DIM
CONV_CHANNELS = D_MODEL // 2
CONV_KERNEL = 31
MOBA_BLOCK = 256
MOBA_TOPK = 3
Q_CHUNK = 32
D_FF = 11008
FFN_CONV_KERNEL = 3
NORM_EPS = 1e-6
LN_EPS = 1e-5
NEG_INF = -1e30
IN_WIDTH = 3 * ATTN_WIDTH + 2 * CONV_CHANNELS + 2 * D_MODEL

kernel_name = "moba_conformer_gated_hybrid"


def rmsnorm(x, g):
    xf = x.astype(jnp.float32)
    y = xf * lax.rsqrt(jnp.mean(xf * xf, axis=-1, keepdims=True) + NORM_EPS)
    return (y * g.astype(jnp.float32)).astype(x.dtype)


def layernorm(x, g, b):
    xf = x.astype(jnp.float32)
    mu = jnp.mean(xf, axis=-1, keepdims=True)
    var = jnp.mean(jnp.square(xf - mu), axis=-1, keepdims=True)
    y = (xf - mu) * lax.rsqrt(var + LN_EPS)
    return (y * g.astype(jnp.float32) + b.astype(jnp.float32)).astype(x.dtype)


def causal_depthwise_conv(x, w, b):
    k_width, channels = w.shape
    y = lax.conv_general_dilated(
        x, w[:, None, :].astype(x.dtype), window_strides=(1,), padding=[(k_width - 1, 0)],
        dimension_numbers=("NWC", "WIO", "NWC"), feature_group_count=channels)
    return y + b.astype(x.dtype)


def alibi_slopes(n_heads):
    h = jnp.arange(1, n_heads + 1, dtype=jnp.float32)
    return jnp.exp2(-8.0 * h / n_heads)


def moba_attention(q, k, v):
    B, S, H, hd = q.shape
    nb = -(-S // MOBA_BLOCK)
    s_pad = nb * MOBA_BLOCK
    pad = s_pad - S

    def prep(t):
        t = jnp.pad(t, ((0, 0), (0, pad), (0, 0), (0, 0)))
        return t.transpose(0, 2, 1, 3)

    q, k, v = prep(q), prep(k), prep(v)
    k_blk = k.reshape(B, H, nb, MOBA_BLOCK, hd)
    v_blk = v.reshape(B, H, nb, MOBA_BLOCK, hd)
    k_mean = jnp.mean(k_blk.astype(jnp.float32), axis=3)

    pos = jnp.arange(s_pad)
    q_block = pos // MOBA_BLOCK
    gate = jnp.einsum("bhsd,bhnd->bhsn", q.astype(jnp.float32), k_mean)
    past = jnp.arange(nb)[None, :] < q_block[:, None]
    gate = jnp.where(past, gate, NEG_INF)
    topk = min(MOBA_TOPK, nb)
    _, sel = lax.top_k(gate, topk)
    sel_valid = sel < q_block[:, None]

    slopes = alibi_slopes(H)
    scale = hd ** -0.5
    b_ix = jnp.arange(B)[:, None, None, None]
    h_ix = jnp.arange(H)[None, :, None, None]
    r = jnp.arange(MOBA_BLOCK)

    def chunk(c):
        start = c * Q_CHUNK
        q_c = lax.dynamic_slice_in_dim(q, start, Q_CHUNK, axis=2)
        sel_c = lax.dynamic_slice_in_dim(sel, start, Q_CHUNK, axis=2)
        valid_c = lax.dynamic_slice_in_dim(sel_valid, start, Q_CHUNK, axis=2)
        q_pos = start + jnp.arange(Q_CHUNK)
        own = start // MOBA_BLOCK
        k_sel = k_blk[b_ix, h_ix, sel_c]
        v_sel = v_blk[b_ix, h_ix, sel_c]
        k_own = lax.dynamic_index_in_dim(k_blk, own, axis=2, keepdims=False)
        v_own = lax.dynamic_index_in_dim(v_blk, own, axis=2, keepdims=False)

        s_sel = jnp.einsum("bhqd,bhqkrd->bhqkr", q_c, k_sel,
                           preferred_element_type=jnp.float32) * scale
        kpos_sel = sel_c[..., None] * MOBA_BLOCK + r
        dist_sel = (q_pos[:, None, None] - kpos_sel).astype(jnp.float32)
        s_sel = jnp.where(valid_c[..., None],
                          s_sel - slopes[:, None, None, None] * dist_sel, NEG_INF)

        s_own = jnp.einsum("bhqd,bhrd->bhqr", q_c, k_own,
                           preferred_element_type=jnp.float32) * scale
        dist_own = (q_pos[:, None] - (own * MOBA_BLOCK + r)[None, :]).astype(jnp.float32)
        s_own = jnp.where(dist_own >= 0, s_own - slopes[:, None, None] * dist_own, NEG_INF)

        logits = jnp.concatenate([s_sel.reshape(B, H, Q_CHUNK, topk * MOBA_BLOCK), s_own], axis=-1)
        p = jax.nn.softmax(logits, axis=-1).astype(v.dtype)
        p_sel = p[..., :topk * MOBA_BLOCK].reshape(B, H, Q_CHUNK, topk, MOBA_BLOCK)
        p_own = p[..., topk * MOBA_BLOCK:]
        return (jnp.einsum("bhqkr,bhqkrd->bhqd", p_sel, v_sel)
                + jnp.einsum("bhqr,bhrd->bhqd", p_own, v_own))

    out = lax.map(chunk, jnp.arange(s_pad // Q_CHUNK))
    out = out.transpose(1, 2, 0, 3, 4).reshape(B, H, s_pad, hd)[:, :, :S]
    return out.transpose(0, 2, 1, 3).reshape(B, S, H * hd)


def setup_inputs(seed: int = 0) -> dict:
    key = jax.random.key(seed)
    ks = jax.random.split(key, 16)
    f32 = jnp.float32

    def nrm(k, shape, scale):
        return jax.random.normal(k, shape, f32) * scale

    return {
        "x": jax.random.normal(ks[0], (BATCH, SEQ, D_MODEL), f32),
        "g_mix": 1.0 + nrm(ks[1], (D_MODEL,), 0.01),
        "w_in": nrm(ks[2], (D_MODEL, IN_WIDTH), D_MODEL ** -0.5),
        "w_conv_dw": nrm(ks[3], (CONV_KERNEL, CONV_CHANNELS), CONV_KERNEL ** -0.5),
        "b_conv_dw": nrm(ks[4], (CONV_CHANNELS,), 0.01),
        "ln_conv_g": 1.0 + nrm(ks[5], (CONV_CHANNELS,), 0.01),
        "ln_conv_b": nrm(ks[6], (CONV_CHANNELS,), 0.01),
        "w_proj_attn": nrm(ks[7], (ATTN_WIDTH, D_MODEL), ATTN_WIDTH ** -0.5),
        "w_proj_conv": nrm(ks[8], (CONV_CHANNELS, D_MODEL), CONV_CHANNELS ** -0.5),
        "w_out": nrm(ks[9], (D_MODEL, D_MODEL), D_MODEL ** -0.5),
        "g_ffn": 1.0 + nrm(ks[10], (D_MODEL,), 0.01),
        "w_up": nrm(ks[11], (D_MODEL, 2 * D_FF), D_MODEL ** -0.5),
        "w_ffn_dw": nrm(ks[12], (FFN_CONV_KERNEL, 2 * D_FF), FFN_CONV_KERNEL ** -0.5),
        "b_ffn_dw": nrm(ks[13], (2 * D_FF,), 0.01),
        "w_down": nrm(ks[14], (D_FF, D_MODEL), D_FF ** -0.5),
        "g_final": 1.0 + nrm(ks[15], (D_MODEL,), 0.01),
    }


def reference(x, g_mix, w_in, w_conv_dw, b_conv_dw, ln_conv_g, ln_conv_b, w_proj_attn,
              w_proj_conv, w_out, g_ffn, w_up, w_ffn_dw, b_ffn_dw, w_down, g_final):
    B, S, _ = x.shape
    h = x
    for _layer in range(DEPTH):
        u = rmsnorm(h, g_mix)
        proj = u @ w_in.astype(u.dtype)
        splits = np.cumsum([ATTN_WIDTH, ATTN_WIDTH, ATTN_WIDTH, CONV_CHANNELS, CONV_CHANNELS, D_MODEL])
        q, k, v, glu_a, glu_b, gate_attn, gate_conv = jnp.split(proj, splits, axis=-1)

        q = q.reshape(B, S, ATTN_HEADS, HEAD_DIM)
        k = k.reshape(B, S, ATTN_HEADS, HEAD_DIM)
        v = v.reshape(B, S, ATTN_HEADS, HEAD_DIM)
        a = moba_attention(q, k, v) @ w_proj_attn.astype(u.dtype)

        c = glu_a * jax.nn.sigmoid(glu_b)
        c = causal_depthwise_conv(c, w_conv_dw, b_conv_dw)
        c = jax.nn.silu(layernorm(c, ln_conv_g, ln_conv_b))
        c = c @ w_proj_conv.astype(u.dtype)

        merged = jax.nn.sigmoid(gate_attn) * a + jax.nn.sigmoid(gate_conv) * c
        h = h + merged @ w_out.astype(u.dtype)

        f = rmsnorm(h, g_ffn)
        up = causal_depthwise_conv(f @ w_up.astype(f.dtype), w_ffn_dw, b_ffn_dw)
        f_gate, f_val = jnp.split(up, 2, axis=-1)
        h = h + (jax.nn.silu(f_gate) * f_val) @ w_down.astype(f.dtype)
    return rmsnorm(h, g_final)
```

```python
import numpy as np
import ml_dtypes
import concourse.bass as bass
import concourse.mybir as mybir
from concourse.bass_utils import run_bass_kernel_spmd

F32 = mybir.dt.float32
BF16 = mybir.dt.bfloat16
U8 = mybir.dt.uint8
AF = mybir.ActivationFunctionType
ALU = mybir.AluOpType
AX = mybir.AxisListType

D = 4096
S = 8192
NCORE = 8
TOK = 1024
EXT = 1152
E0 = S - EXT
H = 16
NBLK = 32
DFF = 11008
NFC = 86
INW = 18432
NEG = -30000.0
SLOPES = [float(2.0 ** (-8.0 * (h + 1) / 16.0)) for h in range(H)]
QSCALE = float(128 ** -0.5)
RNG = [(0, 128), (128, 512), (640, 512)]
HBG0 = [6, 6, 6, 6, 5, 3, 0, 0]
FB_H = [24] * 8 + [20, 20, 16, 12, 8, 0, 0, 0]
KB = 1024

P_WCONV = 0
P_BCONV = 496
P_LNG = 512
P_LNB = 528
P_GFFN = 544
P_GFIN = 576
P_WFFN = 608
P_BFFN = 1124
P_FLAG = 1296
NPRM = 1304
A_DK = 0
A_GMASK = 320
A_GOK = 480
A_BTV = 640
A_BTM = 1152
A_QO = 1664
A_OWN = 1672
A_QO9 = 1832
NATT = 1848


import os as _os
_KCUT = _os.environ.get("KCUT", "")
_KTRACE = _os.environ.get("KTRACE", "")
_KSKIP = _os.environ.get("KSKIP", "")


class Buf:
    __slots__ = ("name", "w", "r")

    def __init__(self, name):
        self.name = name
        self.w = None
        self.r = []


class Op:
    __slots__ = ("eng", "fn", "deps", "sig", "isdma", "semi", "val")


NDS = 8
NCS = 8
ENGS = ("pe", "act", "dve", "pool", "sp")


class Sched:
    def __init__(self):
        self.ops = {e: [] for e in ENGS}
        self.fence = []
        self.last_dma = {e: {} for e in ENGS}
        self.ndma = {e: 0 for e in ENGS}
        self.enabled = True

    def op(self, eng, fn, reads=(), writes=(), dma=False):
        if not self.enabled:
            return None
        o = Op()
        o.eng = eng
        o.fn = fn
        o.isdma = dma
        o.sig = False
        o.semi = None
        o.val = None
        deps = []
        for b in reads:
            if b.w is not None:
                deps.append(b.w)
        for b in writes:
            if b.w is not None:
                deps.append(b.w)
            deps.extend(b.r)
        deps.extend(self.fence)
        dd = []
        seen = set()
        for d in deps:
            if id(d) in seen:
                continue
            seen.add(id(d))
            if d.eng == eng and eng == "pe" and not d.isdma:
                continue
            dd.append(d)
        if dma:
            k = self.ndma[eng]
            self.ndma[eng] += 1
            slot = k % NDS
            o.semi = ("d", eng, slot)
            o.val = 16 * (k // NDS + 1)
            prev = self.last_dma[eng].get(slot)
            if prev is not None and id(prev) not in seen:
                dd.append(prev)
            self.last_dma[eng][slot] = o
            o.sig = True
        o.deps = dd
        for d in dd:
            d.sig = True
        for b in reads:
            b.r.append(o)
        for b in writes:
            b.w = o
            b.r = []
        self.ops[eng].append(o)
        return o

    def barrier(self):
        if not self.enabled:
            return
        f = []
        for e in ENGS:
            for o in reversed(self.ops[e]):
                if not o.isdma:
                    f.append(o)
                    o.sig = True
                    break
            f.extend(self.last_dma[e].values())
        self.fence = f

    def finalize(self):
        for e in ENGS:
            n = 0
            for o in self.ops[e]:
                if o.isdma or not o.sig:
                    continue
                o.semi = ("c", e, n % NCS)
                o.val = n // NCS + 1
                n += 1

    def emit(self, eng, e, sems, final_wait=False):
        waited = {}
        for o in self.ops[eng]:
            for d in o.deps:
                if waited.get(d.semi, 0) >= d.val:
                    continue
                e.wait_ge(sems[d.semi], d.val)
                waited[d.semi] = d.val
            ins = o.fn(e)
            if o.sig:
                ins.then_inc(sems[o.semi], 16 if o.isdma else 1)
            if _KTRACE:
                print("TR", eng, "L%d" % o.fn.__code__.co_firstlineno, "waits", [(d.semi, d.val) for d in o.deps],
                      "inc", (o.semi, o.val) if o.sig else None)
        if final_wait:
            for o in self.last_dma[eng].values():
                if waited.get(o.semi, 0) >= o.val:
                    continue
                e.wait_ge(sems[o.semi], o.val)
                waited[o.semi] = o.val


def build(upto=99, debug=False):
    nc = bass.Bass("TRN2", target_bir_lowering=False)

    def din(name, shape, dt=F32):
        return nc.dram_tensor(name, list(shape), dt, kind="ExternalInput").ap()

    xr = din("xr", [S, D])
    w_in = din("w_in", [D, INW])
    w_pa = din("w_proj_attn", [2048, D])
    w_pc = din("w_proj_conv", [2048, D])
    w_out = din("w_out", [D, D])
    w_up = din("w_up", [D, 2 * DFF])
    w_down = din("w_down", [DFF, D])
    gbc_d = din("gbc", [128, D])
    prm_d = din("prm", [128, NPRM])
    cf32_d = din("cf32", [128, 256])
    identb_d = din("identb", [128, 128], BF16)
    attc_d = din("attc", [128, NATT])
    oh2_d = din("oh2", [128, 4096], BF16)
    out_d = nc.dram_tensor("out", [TOK, D], F32, kind="ExternalOutput").ap()

    def dscr(name, shape, dt):
        if debug:
            return nc.dram_tensor(name, list(shape), dt, kind="ExternalOutput").ap()
        return nc.dram_tensor(name, list(shape), dt).ap()

    kT_d = dscr("kT_s", [H, 128, S], BF16)
    vS_d = dscr("vS_s", [S, 2048], BF16)
    qT_d = dscr("qT_s", [H, 128, EXT], BF16)
    yT_d = dscr("yT_s", [16, 128, EXT], F32)
    sgT_d = dscr("sgT_s", [64, 128, EXT], BF16)
    oT_d = dscr("oT_s", [H, 128, EXT], BF16)
    mT_d = dscr("mT_s", [32, 128, EXT], BF16)
    hT_d = dscr("hT_s", [32, 128, EXT], F32)
    gT_d = dscr("gT_s", [NFC, 128, TOK], BF16)
    h2T_d = dscr("h2T_s", [32, 128, TOK], F32)

    w_in_v = w_in.rearrange("(kc p) n -> p kc n", p=128)
    w_pa_v = w_pa.rearrange("(kc p) n -> p kc n", p=128)
    w_pc_v = w_pc.rearrange("(kc p) n -> p kc n", p=128)
    w_out_v = w_out.rearrange("(kc p) n -> p kc n", p=128)
    w_up_v = w_up.rearrange("(kc p) n -> p kc n", p=128)
    w_down_v = w_down.rearrange("(kc p) n -> p kc n", p=128)

    sc = Sched()
    bufs = {}

    def B(name):
        b = bufs.get(name)
        if b is None:
            b = Buf(name)
            bufs[name] = b
        return b

    with (
        nc.sbuf_tensor("arena", [128, 206 * KB], U8) as arena,
        nc.psum_tensor("ps", [128, 4096], F32) as ps,
        nc.Block() as block,
    ):
        def carve(off, shape, dt):
            n = 1
            for s_ in shape:
                n *= s_
            bs = 4 if dt == F32 else 2
            ap = arena[:, off:off + n * bs].bitcast(dt)
            if len(shape) == 2:
                ap = ap.rearrange("p (a b) -> p a b", a=shape[0])
            return ap

        PB = 188 * KB
        prm = carve(PB, (NPRM,), F32)
        cf32 = carve(PB + 5216, (256,), F32)
        identf = cf32[:, 0:128]
        onesf = cf32[:, 128:256]
        identb = carve(PB + 6240, (128,), BF16)
        kms = carve(PB + 6496, (H, NBLK), F32)
        smalls = carve(PB + 8544, (64,), F32)
        ss_t = [smalls[:, 0:1], smalls[:, 1:2]]
        rs_t = [smalls[:, 2:3], smalls[:, 3:4]]
        wconv = prm[:, P_WCONV:P_WCONV + 496].rearrange("p (c k) -> p c k", k=31)
        bconv = prm[:, P_BCONV:P_BCONV + 16]
        lng = prm[:, P_LNG:P_LNG + 16]
        lnb = prm[:, P_LNB:P_LNB + 16]
        gffn = prm[:, P_GFFN:P_GFFN + 32]
        gfin = prm[:, P_GFIN:P_GFIN + 32]
        wffn = prm[:, P_WFFN:P_WFFN + 516].rearrange("p (c k) -> p c k", k=3)
        bffn = prm[:, P_BFFN:P_BFFN + 172]
        flag = prm[:, P_FLAG:P_FLAG + 1]

        b_prm, b_cf, b_idb = B("prm"), B("cf32"), B("identb")
        sc.op("sp", lambda e: e.dma_start(out=prm, in_=prm_d), writes=[b_prm], dma=True)
        sc.op("sp", lambda e: e.dma_start(out=cf32, in_=cf32_d), writes=[b_cf], dma=True)
        sc.op("sp", lambda e: e.dma_start(out=identb, in_=identb_d), writes=[b_idb], dma=True)

        EA, EB = 0, 1536
        b_EA, b_EB = B("psEA"), B("psEB")

        def mm_ext(e, base, lhs_fn, rhs3, nk):
            last = None
            for kc in range(nk):
                lw = lhs_fn(kc)
                for (t0, n) in RNG:
                    last = e.matmul(ps[:, base + 384 + t0: base + 384 + t0 + n], lhsT=lw,
                                    rhs=rhs3[:, kc, t0:t0 + n], start=(kc == 0), stop=(kc == nk - 1))
            return last

        def wload(dst, src, bdst):
            return sc.op("pool", lambda e: e.dma_start(out=dst, in_=src), writes=[bdst], dma=True)

        def make_uT(row0, ntiles, uT, b_uT, gbc, b_gbc, xts, xss, tag):
            b_xt = [B(tag + "xt0"), B(tag + "xt1")]
            b_xs = [B(tag + "xs0"), B(tag + "xs1")]
            b_ss = [B("ss0"), B("ss1")]
            b_rs = [B("rs0"), B("rs1")]
            b_tp = [B("psb6"), B("psb7")]
            tpv = [ps[:, 3072:3584].bitcast(BF16).rearrange("p (j t) -> p j t", t=128),
                   ps[:, 3584:4096].bitcast(BF16).rearrange("p (j t) -> p j t", t=128)]
            cnt = 0
            for i in range(ntiles):
                s_ = i % 2
                xt, xs, ss, rs = xts[s_], xss[s_], ss_t[s_], rs_t[s_]
                src = xr[row0 + i * 128: row0 + (i + 1) * 128, :]
                sc.op("sp", lambda e, xt=xt, src=src: e.dma_start(out=xt, in_=src), writes=[b_xt[s_]], dma=True)
                sc.op("dve", lambda e, ss=ss: e.memset(ss, 0.0), writes=[b_ss[s_]])
                sc.op("act", lambda e, xt=xt, xs=xs, ss=ss: e.activation(out=xs, in_=xt, func=AF.Square, accum_out=ss),
                      reads=[b_xt[s_]], writes=[b_xs[s_], b_ss[s_]])
                sc.op("dve", lambda e, ss=ss, rs=rs: e.tensor_scalar(out=rs, in0=ss, scalar1=1.0 / D, scalar2=1e-6,
                                                                      op0=ALU.mult, op1=ALU.add),
                      reads=[b_ss[s_]], writes=[b_rs[s_]])
                sc.op("act", lambda e, rs=rs: e.activation(out=rs, in_=rs, func=AF.Sqrt), reads=[b_rs[s_]], writes=[b_rs[s_]])
                sc.op("dve", lambda e, rs=rs: e.reciprocal(out=rs, in_=rs), reads=[b_rs[s_]], writes=[b_rs[s_]])
                sc.op("dve", lambda e, xt=xt, xs=xs, rs=rs: e.scalar_tensor_tensor(
                    out=xs, in0=xt, scalar=rs, in1=gbc, op0=ALU.mult, op1=ALU.mult),
                    reads=[b_xt[s_], b_rs[s_], b_gbc], writes=[b_xs[s_]])
                for q4 in range(4):
                    pb = cnt % 2
                    cnt += 1

                    def tr(e, xs=xs, q4=q4, pb=pb):
                        last = None
                        for j in range(8):
                            kc = q4 * 8 + j
                            last = e.transpose(tpv[pb][:, j, :], xs[:, kc * 128:(kc + 1) * 128], identb)
                        return last
                    sc.op("pe", tr, reads=[b_xs[s_], b_idb], writes=[b_tp[pb]])
                    dst = uT[:, q4 * 8:(q4 + 1) * 8, i * 128:(i + 1) * 128]
                    if q4 % 2 == 0:
                        sc.op("act", lambda e, dst=dst, pb=pb: e.activation(out=dst, in_=tpv[pb], func=AF.Copy),
                              reads=[b_tp[pb]], writes=[b_uT])
                    else:
                        sc.op("dve", lambda e, dst=dst, pb=pb: e.tensor_copy(out=dst, in_=tpv[pb]),
                              reads=[b_tp[pb]], writes=[b_uT])

        sc.enabled = (upto >= 1)
        uT1 = carve(0, (32, 1024), BF16)
        gbc = carve(64 * KB, (D,), F32)
        xts = [carve(80 * KB, (D,), F32), carve(96 * KB, (D,), F32)]
        xss = [carve(112 * KB, (D,), BF16), carve(120 * KB, (D,), BF16)]
        wb = [carve((128 + 16 * i) * KB, (32, 256), BF16) for i in range(3)]
        b_wb = [B("p1wb%d" % i) for i in range(3)]
        kst = [carve(176 * KB, (1024,), BF16), carve(178 * KB, (1024,), BF16)]
        b_kst = [B("kst0"), B("kst1")]
        vst = [carve(180 * KB, (8, 256), BF16), carve(184 * KB, (8, 256), BF16)]
        b_vst = [B("vst0"), B("vst1")]
        b_uT1, b_gbc, b_kms = B("uT1"), B("gbc"), B("kms")
        ubf = carve(197 * KB, (32, NBLK), F32)
        ubhl = carve(201 * KB, (32, 64), BF16)
        ubt = carve(PB + 8800, (64,), F32)[:, 0:32]
        b_ubf, b_ubhl, b_ubt = B("ubf"), B("ubhl"), B("ubt")
        sc.op("sp", lambda e: e.dma_start(out=gbc, in_=gbc_d), writes=[b_gbc], dma=True)
        kacc = [(0, B("psK0")), (1024, B("psK1"))]
        vacc = [(2048, B("psV0")), (2560, B("psV1"))]
        wcnt = 0
        kcnt = 0
        vcnt = 0
        for g in range(8):
            make_uT(g * 1024, 8, uT1, b_uT1, gbc, b_gbc, xts, xss, "p1")
            if _KCUT == "a":
                sc.enabled = False
            sc.op("dve", lambda e, g=g: e.tensor_reduce(out=ubf[:, :, g * 4:(g + 1) * 4],
                                                     in_=uT1.rearrange("p k (b t) -> p k b t", t=256), axis=AX.X, op=ALU.add),
                  reads=[b_uT1], writes=[b_ubf])
            if g == 7:
                sc.op("dve", lambda e: e.tensor_scalar(out=ubf, in0=ubf, scalar1=1.0 / 256.0, scalar2=None, op0=ALU.mult),
                      reads=[b_ubf], writes=[b_ubf])
                sc.op("dve", lambda e: e.tensor_copy(out=ubhl[:, :, 0:32], in_=ubf), reads=[b_ubf], writes=[b_ubhl])
                sc.op("dve", lambda e: e.tensor_tensor(out=ubf, in0=ubf, in1=ubhl[:, :, 0:32], op=ALU.subtract),
                      reads=[b_ubf, b_ubhl], writes=[b_ubf])
                sc.op("dve", lambda e: e.tensor_copy(out=ubhl[:, :, 32:64], in_=ubf), reads=[b_ubf], writes=[b_ubhl])
            for hb in range(8):
                if g < HBG0[hb]:
                    continue
                ws = wcnt % 3
                wcnt += 1
                wload(wb[ws], w_in_v[:, :, 2048 + hb * 256: 2048 + (hb + 1) * 256], b_wb[ws])
                for hh in range(2):
                    h = hb * 2 + hh
                    base, b_acc = kacc[kcnt % 2]
                    ks = kcnt % 2
                    kcnt += 1

                    def kmm(e, ws=ws, hh=hh, base=base):
                        last = None
                        for kc in range(32):
                            lw = wb[ws][:, kc, hh * 128:(hh + 1) * 128]
                            for half in range(2):
                                last = e.matmul(ps[:, base + half * 512: base + (half + 1) * 512], lhsT=lw,
                                                rhs=uT1[:, kc, half * 512:(half + 1) * 512],
                                                start=(kc == 0), stop=(kc == 31))
                        return last
                    sc.op("pe", kmm, reads=[b_wb[ws], b_uT1], writes=[b_acc])
                    if "c" not in _KSKIP:
                        sc.op("act", lambda e, ks=ks, base=base: e.activation(out=kst[ks], in_=ps[:, base:base + 1024], func=AF.Copy),
                              reads=[b_acc], writes=[b_kst[ks]])
                    if g == 7:
                        def kmean_mm(e, ws=ws, hh=hh):
                            last = None
                            for kc in range(32):
                                last = e.matmul(ps[:, 2560:2624], lhsT=wb[ws][:, kc, hh * 128:(hh + 1) * 128], rhs=ubhl[:, kc, :],
                                                start=(kc == 0), stop=(kc == 31))
                            return last
                        sc.op("pe", kmean_mm, reads=[b_wb[ws], b_ubhl], writes=[B("psV1")])
                        sc.op("act", lambda e: e.activation(out=ubt, in_=ps[:, 2592:2624], func=AF.Copy), reads=[B("psV1")], writes=[b_ubt])
                        sc.op("dve", lambda e, h=h: e.tensor_tensor(out=kms[:, h, :], in0=ps[:, 2560:2592], in1=ubt, op=ALU.add),
                              reads=[B("psV1"), b_ubt], writes=[b_kms])
                    if "d" not in _KSKIP:
                        sc.op("sp", lambda e, ks=ks, h=h, g=g: e.dma_start(out=kT_d[h, :, g * 1024:(g + 1) * 1024], in_=kst[ks]),
                              reads=[b_kst[ks]], writes=[B("kT%d" % h)], dma=True)
            if _KCUT == "b":
                sc.enabled = False
            for vb in range(8):
                if g < HBG0[vb]:
                    continue
                ws = wcnt % 3
                wcnt += 1
                wload(wb[ws], w_in_v[:, :, 4096 + vb * 256: 4096 + (vb + 1) * 256], b_wb[ws])
                vs = vb % 2
                for t in range(8):
                    base, b_acc = vacc[vcnt % 2]
                    vcnt += 1

                    def vmm(e, ws=ws, t=t, base=base):
                        last = None
                        for kc in range(32):
                            last = e.matmul(ps[:, base:base + 256], lhsT=uT1[:, kc, t * 128:(t + 1) * 128],
                                            rhs=wb[ws][:, kc, :], start=(kc == 0), stop=(kc == 31))
                        return last
                    sc.op("pe", vmm, reads=[b_wb[ws], b_uT1], writes=[b_acc])
                    if t % 2 == 0:
                        sc.op("act", lambda e, vs=vs, t=t, base=base: e.activation(out=vst[vs][:, t, :], in_=ps[:, base:base + 256], func=AF.Copy),
                              reads=[b_acc], writes=[b_vst[vs]])
                    else:
                        sc.op("dve", lambda e, vs=vs, t=t, base=base: e.tensor_copy(out=vst[vs][:, t, :], in_=ps[:, base:base + 256]),
                              reads=[b_acc], writes=[b_vst[vs]])
                dst = vS_d[g * 1024:(g + 1) * 1024, vb * 256:(vb + 1) * 256].rearrange("(t p) c -> p t c", p=128)
                sc.op("sp", lambda e, vs=vs, dst=dst: e.dma_start(out=dst, in_=vst[vs]),
                      reads=[b_vst[vs]], writes=[B("vS")], dma=True)
            if _KCUT == "c":
                sc.enabled = False
        sc.barrier()

        sc.enabled = (upto >= 2)
        uT2 = carve(0, (32, EXT), BF16)
        b_uT2 = B("uT2")
        gbc2 = carve(72 * KB, (D,), F32)
        b_gbc2 = B("gbc2")
        sc.op("sp", lambda e: e.dma_start(out=gbc2, in_=gbc_d), writes=[b_gbc2], dma=True)
        xts2 = [carve(88 * KB, (D,), F32), carve(104 * KB, (D,), F32)]
        xss2 = [carve(120 * KB, (D,), BF16), carve(128 * KB, (D,), BF16)]
        make_uT(E0, 9, uT2, b_uT2, gbc2, b_gbc2, xts2, xss2, "p2")
        sc.barrier()
        wb2 = [carve((72 + 16 * i) * KB, (32, 256), BF16) for i in range(4)]
        b_wb2 = [B("p2wb%d" % i) for i in range(4)]
        qst = [carve(136 * KB, (EXT,), BF16), carve(139 * KB, (EXT,), BF16)]
        b_qst = [B("qst0"), B("qst1")]
        sgb = carve(142 * KB, (EXT,), F32)
        b_sgb = B("sgb")
        cpad = [carve(147 * KB, (EXT + 30,), F32), carve(152 * KB, (EXT + 30,), F32)]
        b_cpad = [B("cpad0"), B("cpad1")]
        yb = [carve(157 * KB, (EXT,), F32), carve(162 * KB, (EXT,), F32)]
        b_yb = [B("yb0"), B("yb1")]
        sgst = [carve(167 * KB, (EXT,), BF16), carve(170 * KB, (EXT,), BF16)]
        b_sgst = [B("sgst0"), B("sgst1")]
        for i in range(2):
            sc.op("dve", lambda e, i=i: e.memset(cpad[i][:, 0:30], 0.0), writes=[b_cpad[i]])
        accs = [(EA, b_EA), (EB, b_EB)]
        acnt = 0
        wcnt = 0
        for hb in range(8):
            ws = wcnt % 4
            wcnt += 1
            wload(wb2[ws], w_in_v[:, :, hb * 256:(hb + 1) * 256], b_wb2[ws])
            for hh in range(2):
                h = hb * 2 + hh
                base, b_acc = accs[acnt % 2]
                acnt += 1
                qs = h % 2
                sc.op("pe", lambda e, ws=ws, hh=hh, base=base: mm_ext(
                    e, base, lambda kc: wb2[ws][:, kc, hh * 128:(hh + 1) * 128], uT2, 32),
                    reads=[b_wb2[ws], b_uT2], writes=[b_acc])
                sc.op("act", lambda e, qs=qs, base=base: e.activation(
                    out=qst[qs], in_=ps[:, base + 384: base + 1536], func=AF.Copy, scale=QSCALE),
                    reads=[b_acc], writes=[b_qst[qs]])
                sc.op("sp", lambda e, qs=qs, h=h: e.dma_start(out=qT_d[h], in_=qst[qs]),
                      reads=[b_qst[qs]], writes=[B("qT%d" % h)], dma=True)
        for cb in range(8):
            wsa = wcnt % 4
            wcnt += 1
            wload(wb2[wsa], w_in_v[:, :, 6144 + cb * 256: 6144 + (cb + 1) * 256], b_wb2[wsa])
            wsb = wcnt % 4
            wcnt += 1
            wload(wb2[wsb], w_in_v[:, :, 8192 + cb * 256: 8192 + (cb + 1) * 256], b_wb2[wsb])
            for c2 in range(2):
                cc = cb * 2 + c2
                cs = cc % 2
                sc.op("pe", lambda e, wsb=wsb, c2=c2: mm_ext(
                    e, EB, lambda kc: wb2[wsb][:, kc, c2 * 128:(c2 + 1) * 128], uT2, 32),
                    reads=[b_wb2[wsb], b_uT2], writes=[b_EB])
                sc.op("act", lambda e: e.activation(out=sgb, in_=ps[:, EB + 384:EB + 1536], func=AF.Sigmoid),
                      reads=[b_EB], writes=[b_sgb])
                sc.op("pe", lambda e, wsa=wsa, c2=c2: mm_ext(
                    e, EA, lambda kc: wb2[wsa][:, kc, c2 * 128:(c2 + 1) * 128], uT2, 32),
                    reads=[b_wb2[wsa], b_uT2], writes=[b_EA])
                sc.op("dve", lambda e, cs=cs: e.tensor_tensor(out=cpad[cs][:, 30:30 + EXT], in0=ps[:, EA + 384:EA + 1536],
                                                              in1=sgb, op=ALU.mult),
                      reads=[b_EA, b_sgb], writes=[b_cpad[cs]])
                sc.op("act", lambda e, cs=cs, cc=cc: e.activation(
                    out=yb[cs], in_=cpad[cs][:, 0:EXT], func=AF.Identity,
                    scale=wconv[:, cc, 0:1], bias=bconv[:, cc:cc + 1]),
                    reads=[b_cpad[cs], b_prm], writes=[b_yb[cs]])
                for k in range(1, 31):
                    sc.op("dve", lambda e, cs=cs, cc=cc, k=k: e.scalar_tensor_tensor(
                        out=yb[cs], in0=cpad[cs][:, k:k + EXT], scalar=wconv[:, cc, k:k + 1], in1=yb[cs],
                        op0=ALU.mult, op1=ALU.add), reads=[b_cpad[cs], b_yb[cs]], writes=[b_yb[cs]])
                sc.op("sp", lambda e, cs=cs, cc=cc: e.dma_start(out=yT_d[cc], in_=yb[cs]),
                      reads=[b_yb[cs]], writes=[B("yT%d" % cc)], dma=True)
        for gb in range(32):
            ws = wcnt % 4
            wcnt += 1
            wload(wb2[ws], w_in_v[:, :, 10240 + gb * 256: 10240 + (gb + 1) * 256], b_wb2[ws])
            for c2 in range(2):
                gc = gb * 2 + c2
                base, b_acc = accs[acnt % 2]
                acnt += 1
                gs = gc % 2
                sc.op("pe", lambda e, ws=ws, c2=c2, base=base: mm_ext(
                    e, base, lambda kc: wb2[ws][:, kc, c2 * 128:(c2 + 1) * 128], uT2, 32),
                    reads=[b_wb2[ws], b_uT2], writes=[b_acc])
                sc.op("act", lambda e, gs=gs, base=base: e.activation(
                    out=sgst[gs], in_=ps[:, base + 384: base + 1536], func=AF.Sigmoid),
                    reads=[b_acc], writes=[b_sgst[gs]])
                sc.op("sp", lambda e, gs=gs, gc=gc: e.dma_start(out=sgT_d[gc], in_=sgst[gs]),
                      reads=[b_sgst[gs]], writes=[B("sgT%d" % gc)], dma=True)
        sc.barrier()

        sc.enabled = (upto >= 3)
        kTh = [carve(0, (S,), BF16), carve(16 * KB, (S,), BF16)]
        Va = [carve(32 * KB, (64, 129), BF16), carve(49 * KB, (64, 129), BF16)]
        qTh = [carve(66 * KB, (EXT,), BF16), carve(69 * KB, (EXT,), BF16)]
        oTh = [carve(72 * KB, (EXT,), BF16), carve(75 * KB, (EXT,), BF16)]
        attc = carve(80 * KB, (NATT,), F32)
        oh2 = carve(88 * KB, (32, 128), BF16)
        biask = [carve(96 * KB, (64,), F32), carve(96 * KB + 256, (64,), F32)]
        aqh = [carve(97 * KB, (9,), F32), carve(97 * KB + 64, (9,), F32)]
        causb = carve(98 * KB, (2, 256), BF16)
        onesb = carve(99 * KB, (128,), BF16)
        kmhi = carve(105 * KB, (H, NBLK), BF16)
        kmlo = carve(106 * KB, (H, NBLK), BF16)
        kmf = carve(107 * KB, (H, NBLK), F32)
        tmp = carve(109 * KB, (768,), F32)
        m8 = [tmp[:, 8:16], tmp[:, 16:24]]
        gm = [tmp[:, 32:64], tmp[:, 64:96]]
        incl = [tmp[:, 96:128], tmp[:, 128:160]]
        row = [tmp[:, 160:192], tmp[:, 192:224]]
        dtm = [tmp[:, 224:256], tmp[:, 256:288]]
        rhl = [carve(112 * KB, (64,), BF16), carve(112 * KB + 128, (64,), BF16)]
        coefT = [carve(113 * KB, (EXT,), BF16), carve(116 * KB, (EXT,), BF16)]
        pT = [carve(120 * KB, (640,), BF16), carve(122 * KB, (640,), BF16), carve(124 * KB, (640,), BF16)]
        rdn = [carve(126 * KB, (640,), F32), carve(129 * KB, (640,), F32)]
        dk = attc[:, A_DK:A_DK + 64]
        gmask = attc[:, A_GMASK:A_GMASK + 160]
        gok = attc[:, A_GOK:A_GOK + 160]
        btm = attc[:, A_BTM:A_BTM + 512].rearrange("p (a b) -> p a b", a=2)
        own01 = attc[:, A_OWN:A_OWN + 160]
        qo9 = attc[:, A_QO9:A_QO9 + 9]
        b_attc, b_oh2 = B("attc"), B("oh2")
        sc.op("sp", lambda e: e.dma_start(out=attc, in_=attc_d), writes=[b_attc], dma=True)
        sc.op("sp", lambda e: e.dma_start(out=oh2, in_=oh2_d.rearrange("p (a b) -> p a b", a=32)), writes=[b_oh2], dma=True)
        b_kTh = [B("kTh0"), B("kTh1")]
        b_Va = [B("Va0"), B("Va1")]
        b_qTh = [B("qTh0"), B("qTh1")]
        b_oTh = [B("oTh0"), B("oTh1")]
        b_pT = [B("pT%d" % i) for i in range(3)]
        b_rdn = [B("rdn0"), B("rdn1")]
        bk = [B("psbank%d" % i) for i in range(8)]
        b_km, b_cau = B("kmhl"), B("causb")
        sc.op("dve", lambda e: e.tensor_copy(out=causb, in_=btm), reads=[b_attc], writes=[b_cau])
        sc.op("dve", lambda e: e.memset(onesb, 1.0), writes=[b_cau])
        sc.op("dve", lambda e: e.tensor_copy(out=kmf, in_=kms), reads=[b_kms], writes=[b_km])
        sc.op("dve", lambda e: e.tensor_copy(out=kmhi, in_=kmf), reads=[b_km], writes=[b_km])
        sc.op("dve", lambda e: e.tensor_tensor(out=kmf, in0=kmf, in1=kmhi, op=ALU.subtract), reads=[b_km], writes=[b_km])
        sc.op("dve", lambda e: e.tensor_copy(out=kmlo, in_=kmf), reads=[b_km], writes=[b_km])
        psg = ps[:, 3072:3104]
        pst = ps[:, 3584:4096].bitcast(BF16)[0:64, 0:128]

        def head_loads(h):
            s_ = h % 2
            fb = FB_H[h]
            nck = 64 - 2 * fb
            sc.op("sp", lambda e: e.dma_start(out=kTh[s_][:, 0:S - fb * 256], in_=kT_d[h, :, fb * 256:S]),
                  reads=[B("kT%d" % h)], writes=[b_kTh[s_]], dma=True)
            sc.op("sp", lambda e: e.dma_start(out=Va[s_][:, 0:nck, 0:128],
                                              in_=vS_d[fb * 256:S, h * 128:(h + 1) * 128].rearrange("(c p) d -> p c d", p=128)),
                  reads=[B("vS")], writes=[b_Va[s_]], dma=True)
            sc.op("sp", lambda e: e.dma_start(out=qTh[s_], in_=qT_d[h]), reads=[B("qT%d" % h)], writes=[b_qTh[s_]], dma=True)

        PASSES = [
            dict(t0=0, n=640, rng=[(0, 128), (128, 512)], sb=[0, 1024], off=384, ob=2048, db=3072,
                 sbk=[[0, 1], [2, 3]], obk=[4, 5], dbk=[6, 7], kend=60, groups=[(0, 0, 128, 128), (1, 128, 256, 0), (2, 384, 256, 0)]),
            dict(t0=640, n=512, rng=[(640, 512)], sb=[0, 512], off=-640, ob=1024, db=1536,
                 sbk=[[0], [1]], obk=[2], dbk=[3], kend=64, groups=[(3, 640, 256, 0), (4, 896, 256, 0)]),
        ]
        head_loads(0)
        tcnt = 0
        pcnt = 0
        rcnt = 0
        for h in range(H):
            hs = h % 2
            sl = SLOPES[h]
            if h + 1 < H:
                head_loads(h + 1)
            b_hc = B("hc%d" % hs)
            sc.op("dve", lambda e, hs=hs, sl=sl: e.tensor_scalar(out=biask[hs], in0=dk, scalar1=-sl, scalar2=None, op0=ALU.mult),
                  reads=[b_attc], writes=[b_hc])
            sc.op("dve", lambda e, hs=hs, sl=sl: e.tensor_scalar(out=aqh[hs], in0=qo9, scalar1=-sl, scalar2=None, op0=ALU.mult),
                  reads=[b_attc], writes=[b_hc])
            b_coef = B("coefT%d" % hs)
            for ti in range(9):
                G = 0 if ti == 0 else (ti - 1) // 2 + 1
                ts = tcnt % 2
                tcnt += 1
                b_t = B("gt%d" % ts)
                tc0 = ti * 128

                def gmm(e, hs=hs, h=h, tc0=tc0):
                    e.matmul(psg, lhsT=qTh[hs][:, tc0:tc0 + 128], rhs=kmhi[:, h, :], start=True, stop=False)
                    return e.matmul(psg, lhsT=qTh[hs][:, tc0:tc0 + 128], rhs=kmlo[:, h, :], start=False, stop=True)
                sc.op("pe", gmm, reads=[b_qTh[hs], b_km], writes=[bk[6]])
                sc.op("dve", lambda e, ts=ts, G=G: e.tensor_tensor(out=gm[ts], in0=psg, in1=gmask[:, G * 32:(G + 1) * 32], op=ALU.add),
                      reads=[bk[6], b_attc], writes=[b_t])
                sc.op("dve", lambda e, ts=ts: e.max(out=m8[ts], in_=gm[ts]), reads=[b_t], writes=[b_t])
                sc.op("dve", lambda e, ts=ts: e.tensor_scalar(out=incl[ts], in0=gm[ts], scalar1=m8[ts][:, 2:3], scalar2=None, op0=ALU.is_ge),
                      reads=[b_t], writes=[b_t])
                sc.op("dve", lambda e, ts=ts, G=G: e.tensor_tensor(out=incl[ts], in0=incl[ts], in1=gok[:, G * 32:(G + 1) * 32], op=ALU.mult),
                      reads=[b_t, b_attc], writes=[b_t])
                sc.op("dve", lambda e, ts=ts, G=G: e.tensor_tensor(out=incl[ts], in0=incl[ts], in1=own01[:, G * 32:(G + 1) * 32], op=ALU.add),
                      reads=[b_t, b_attc], writes=[b_t])
                sc.op("dve", lambda e, ts=ts: e.tensor_scalar(out=row[ts], in0=incl[ts], scalar1=-1.0, scalar2=-NEG, op0=ALU.add, op1=ALU.mult),
                      reads=[b_t], writes=[b_t])
                sc.op("dve", lambda e, ts=ts, hs=hs, ti=ti: e.tensor_scalar(out=row[ts], in0=row[ts], scalar1=aqh[hs][:, ti:ti + 1], scalar2=None, op0=ALU.add),
                      reads=[b_t, b_hc], writes=[b_t])
                sc.op("dve", lambda e, ts=ts: e.tensor_copy(out=rhl[ts][:, 0:32], in_=row[ts]), reads=[b_t], writes=[b_t])
                sc.op("dve", lambda e, ts=ts: e.tensor_tensor(out=dtm[ts], in0=row[ts], in1=rhl[ts][:, 0:32], op=ALU.subtract),
                      reads=[b_t], writes=[b_t])
                sc.op("dve", lambda e, ts=ts: e.tensor_copy(out=rhl[ts][:, 32:64], in_=dtm[ts]), reads=[b_t], writes=[b_t])
                sc.op("pe", lambda e, ts=ts: e.transpose(pst, rhl[ts], identb), reads=[b_t, b_idb], writes=[bk[7]])
                sc.op("act", lambda e, hs=hs, tc0=tc0: e.activation(out=coefT[hs][0:64, tc0:tc0 + 128], in_=pst, func=AF.Copy),
                      reads=[bk[7]], writes=[b_coef])

            def attn_pass(P, h=h, hs=hs, b_hc=b_hc, b_coef=b_coef):
                nonlocal pcnt, rcnt
                fb = FB_H[h]
                kcs = list(range(2 * fb, P["kend"]))
                nk = len(kcs)
                t0, n, off = P["t0"], P["n"], P["off"]
                slot_of = {}

                def emit_S(i):
                    kc = kcs[i]
                    sl_ = i % 2
                    slot_of[i] = sl_
                    sb = P["sb"][sl_]
                    nblk = kc // 2
                    kk = (kc - 2 * fb) * 128
                    diag = [g_ for g_ in P["groups"] if 27 + g_[0] == nblk]

                    def smm(e):
                        last = None
                        for (r0, rn) in P["rng"]:
                            c_ = sb + off + r0
                            e.matmul(ps[:, c_:c_ + rn], lhsT=kTh[hs][:, kk:kk + 128], rhs=qTh[hs][:, r0:r0 + rn], start=True, stop=False)
                        for (r0, rn) in P["rng"]:
                            c_ = sb + off + r0
                            last = e.matmul(ps[:, c_:c_ + rn], lhsT=oh2[0:64, nblk, :], rhs=coefT[hs][0:64, r0:r0 + rn], start=False, stop=True)
                        for (G_, gc0, gW, gb0) in diag:
                            c_ = sb + off + gc0
                            last = e.matmul(ps[:, c_:c_ + gW], lhsT=identb, rhs=causb[:, kc % 2, gb0:gb0 + gW], start=False, stop=True,
                                            skip_group_check=True)
                        return last
                    sbufs = [bk[b_] for b_ in P["sbk"][sl_]]
                    sc.op("pe", smm, reads=[b_kTh[hs], b_qTh[hs], b_oh2, b_coef, b_idb, b_cau], writes=sbufs)
                    pi = pcnt_box[0] % 3
                    pcnt_box[0] += 1
                    slot_of[("p", i)] = pi
                    c_ = sb + off + t0
                    sc.op("act", lambda e, pi=pi, c_=c_, kc=kc: e.activation(out=pT[pi][:, 0:n], in_=ps[:, c_:c_ + n], func=AF.Exp,
                                                                          bias=biask[hs][:, kc:kc + 1]),
                          reads=sbufs + [b_hc], writes=[b_pT[pi]])

                def emit_PV(i):
                    kc = kcs[i]
                    pi = slot_of[("p", i)]
                    vi = kc - 2 * fb

                    def pvm(e):
                        last = None
                        for (r0, rn) in P["rng"]:
                            c_ = P["ob"] + off + r0
                            e.matmul(ps[:, c_:c_ + rn], lhsT=Va[hs][:, vi, 0:128], rhs=pT[pi][:, r0 - t0:r0 - t0 + rn],
                                     start=(i == 0), stop=(i == nk - 1))
                        for (r0, rn) in P["rng"]:
                            c_ = P["db"] + off + r0
                            last = e.matmul(ps[:, c_:c_ + rn], lhsT=onesb, rhs=pT[pi][:, r0 - t0:r0 - t0 + rn],
                                            start=(i == 0), stop=(i == nk - 1))
                        return last
                    sc.op("pe", pvm, reads=[b_pT[pi], b_Va[hs], b_cau], writes=[bk[b_] for b_ in P["obk"] + P["dbk"]])

                emit_S(0)
                for i in range(nk):
                    if i + 1 < nk:
                        emit_S(i + 1)
                    emit_PV(i)
                rs_ = rcnt % 2
                rcnt += 1
                dcol = P["db"] + off + t0
                ocol = P["ob"] + off + t0
                sc.op("dve", lambda e, rs_=rs_, dcol=dcol: e.reciprocal(out=rdn[rs_][:, 0:n], in_=ps[:, dcol:dcol + n]),
                      reads=[bk[b_] for b_ in P["dbk"]], writes=[b_rdn[rs_]])
                sc.op("dve", lambda e, rs_=rs_, ocol=ocol: e.tensor_tensor(out=oTh[hs][:, t0:t0 + n], in0=ps[:, ocol:ocol + n], in1=rdn[rs_][:, 0:n], op=ALU.mult),
                      reads=[bk[b_] for b_ in P["obk"]] + [b_rdn[rs_]], writes=[b_oTh[hs]])

            pcnt_box = [pcnt]
            for P in PASSES:
                attn_pass(P)
            pcnt = pcnt_box[0]
            sc.op("sp", lambda e, hs=hs, h=h: e.dma_start(out=oT_d[h], in_=oTh[hs]), reads=[b_oTh[hs]], writes=[B("oT")], dma=True)
        sc.barrier()

        sc.enabled = (upto >= 4)
        ySB = carve(0, (16, EXT), F32)
        zT = carve(72 * KB, (16, EXT), BF16)
        mean = carve(108 * KB, (EXT,), F32)
        rstd = carve(113 * KB, (EXT,), F32)
        tln = carve(118 * KB, (EXT,), F32)
        ysq = carve(123 * KB, (EXT,), F32)
        b_ySB, b_zT, b_mean, b_rstd, b_tln, b_ysq = B("ySB"), B("zT"), B("mean"), B("rstd"), B("tln"), B("ysq")
        sc.op("sp", lambda e: e.dma_start(out=ySB, in_=yT_d.rearrange("c p t -> p c t")),
              reads=[B("yT%d" % c) for c in range(16)], writes=[b_ySB], dma=True)

        def ones_mm(e, base, src, first, last_):
            last = None
            for (t0, n) in RNG:
                last = e.matmul(ps[:, base + 384 + t0: base + 384 + t0 + n], lhsT=onesf, rhs=src[:, t0:t0 + n],
                                start=first, stop=last_)
            return last
        for cc in range(16):
            sc.op("pe", lambda e, cc=cc: ones_mm(e, EA, ySB[:, cc, :], cc == 0, cc == 15), reads=[b_ySB, b_cf], writes=[b_EA])
            sc.op("act", lambda e, cc=cc: e.activation(out=ysq, in_=ySB[:, cc, :], func=AF.Square), reads=[b_ySB], writes=[b_ysq])
            sc.op("pe", lambda e, cc=cc: ones_mm(e, EB, ysq, cc == 0, cc == 15), reads=[b_ysq, b_cf], writes=[b_EB])
        sc.op("dve", lambda e: e.tensor_scalar(out=mean, in0=ps[:, EA + 384:EA + 1536], scalar1=1.0 / 2048, scalar2=None, op0=ALU.mult),
              reads=[b_EA], writes=[b_mean])
        sc.op("dve", lambda e: e.tensor_tensor(out=tln, in0=mean, in1=mean, op=ALU.mult), reads=[b_mean], writes=[b_tln])
        sc.op("dve", lambda e: e.scalar_tensor_tensor(out=rstd, in0=ps[:, EB + 384:EB + 1536], scalar=1.0 / 2048, in1=tln,
                                                      op0=ALU.mult, op1=ALU.subtract), reads=[b_EB, b_tln], writes=[b_rstd])
        sc.op("dve", lambda e: e.tensor_scalar(out=rstd, in0=rstd, scalar1=1e-5, scalar2=None, op0=ALU.add),
              reads=[b_rstd], writes=[b_rstd])
        sc.op("act", lambda e: e.activation(out=rstd, in_=rstd, func=AF.Sqrt), reads=[b_rstd], writes=[b_rstd])
        sc.op("dve", lambda e: e.reciprocal(out=rstd, in_=rstd), reads=[b_rstd], writes=[b_rstd])
        for cc in range(16):
            sc.op("dve", lambda e, cc=cc: e.tensor_tensor(out=tln, in0=ySB[:, cc, :], in1=mean, op=ALU.subtract),
                  reads=[b_ySB, b_mean], writes=[b_tln])
            sc.op("dve", lambda e: e.tensor_tensor(out=tln, in0=tln, in1=rstd, op=ALU.mult), reads=[b_tln, b_rstd], writes=[b_tln])
            sc.op("act", lambda e, cc=cc: e.activation(out=zT[:, cc, :], in_=tln, func=AF.Silu, scale=lng[:, cc:cc + 1], bias=lnb[:, cc:cc + 1]),
                  reads=[b_tln, b_prm], writes=[b_zT])
        sc.barrier()

        sc.enabled = (upto >= 5)
        oTa = carve(0, (16, EXT), BF16)
        b_oTa = B("oTa")
        sc.op("sp", lambda e: e.dma_start(out=oTa, in_=oT_d.rearrange("h p t -> p h t")), reads=[B("oT")], writes=[b_oTa], dma=True)
        wpa = [carve(36 * KB, (16, 256), BF16), carve(44 * KB, (16, 256), BF16)]
        wpc = [carve(52 * KB, (16, 256), BF16), carve(60 * KB, (16, 256), BF16)]
        b_wpa, b_wpc = [B("wpa0"), B("wpa1")], [B("wpc0"), B("wpc1")]
        sga = [carve(108 * KB, (EXT,), BF16), carve(111 * KB, (EXT,), BF16)]
        sgc = [carve(114 * KB, (EXT,), BF16), carve(117 * KB, (EXT,), BF16)]
        b_sga, b_sgc = [B("sga0"), B("sga1")], [B("sgc0"), B("sgc1")]
        t1 = carve(120 * KB, (EXT,), F32)
        t2 = carve(125 * KB, (EXT,), F32)
        b_t1, b_t2 = B("t1"), B("t2")
        mst = [carve(130 * KB, (EXT,), BF16), carve(133 * KB, (EXT,), BF16)]
        b_mst = [B("mst0"), B("mst1")]
        for db in range(16):
            s_ = db % 2
            wload(wpa[s_], w_pa_v[:, :, db * 256:(db + 1) * 256], b_wpa[s_])
            wload(wpc[s_], w_pc_v[:, :, db * 256:(db + 1) * 256], b_wpc[s_])
            for c2 in range(2):
                dc = db * 2 + c2
                ms = dc % 2
                sc.op("sp", lambda e, ms=ms, dc=dc: e.dma_start(out=sga[ms], in_=sgT_d[dc]), reads=[B("sgT%d" % dc)], writes=[b_sga[ms]], dma=True)
                sc.op("sp", lambda e, ms=ms, dc=dc: e.dma_start(out=sgc[ms], in_=sgT_d[32 + dc]), reads=[B("sgT%d" % (32 + dc))], writes=[b_sgc[ms]], dma=True)
                sc.op("pe", lambda e, s_=s_, c2=c2: mm_ext(e, EA, lambda kc: wpa[s_][:, kc, c2 * 128:(c2 + 1) * 128], oTa, 16),
                      reads=[b_wpa[s_], b_oTa], writes=[b_EA])
                sc.op("pe", lambda e, s_=s_, c2=c2: mm_ext(e, EB, lambda kc: wpc[s_][:, kc, c2 * 128:(c2 + 1) * 128], zT, 16),
                      reads=[b_wpc[s_], b_zT], writes=[b_EB])
                sc.op("dve", lambda e, ms=ms: e.tensor_tensor(out=t1, in0=ps[:, EA + 384:EA + 1536], in1=sga[ms], op=ALU.mult),
                      reads=[b_EA, b_sga[ms]], writes=[b_t1])
                sc.op("dve", lambda e, ms=ms: e.tensor_tensor(out=t2, in0=ps[:, EB + 384:EB + 1536], in1=sgc[ms], op=ALU.mult),
                      reads=[b_EB, b_sgc[ms]], writes=[b_t2])
                sc.op("dve", lambda e, ms=ms: e.tensor_tensor(out=mst[ms], in0=t1, in1=t2, op=ALU.add),
                      reads=[b_t1, b_t2], writes=[b_mst[ms]])
                sc.op("sp", lambda e, ms=ms, dc=dc: e.dma_start(out=mT_d[dc], in_=mst[ms]), reads=[b_mst[ms]], writes=[B("mT")], dma=True)
        sc.barrier()

        sc.enabled = (upto >= 6)
        mTs = carve(0, (32, EXT), BF16)
        b_mTs = B("mTs")
        sc.op("sp", lambda e: e.dma_start(out=mTs, in_=mT_d.rearrange("c p t -> p c t")), reads=[B("mT")], writes=[b_mTs], dma=True)
        wo = [carve((72 + 16 * i) * KB, (32, 256), BF16) for i in range(3)]
        b_wo = [B("wo%d" % i) for i in range(3)]
        xe = [carve(120 * KB, (9, 128), F32), carve(125 * KB, (9, 128), F32)]
        b_xe = [B("xe0"), B("xe1")]
        hst = [carve(130 * KB, (EXT,), F32), carve(135 * KB, (EXT,), F32)]
        b_hst = [B("hst0"), B("hst1")]
        sq = carve(140 * KB, (EXT,), F32)
        b_sq = B("sq")
        rstdf = carve(145 * KB, (EXT,), F32)
        b_rstdf = B("rstdf")
        for ob in range(16):
            s_ = ob % 3
            wload(wo[s_], w_out_v[:, :, ob * 256:(ob + 1) * 256], b_wo[s_])
            for c2 in range(2):
                dc = ob * 2 + c2
                xs_ = dc % 2
                src = xr[E0:S, dc * 128:(dc + 1) * 128].rearrange("(i p) d -> p i d", p=128)
                sc.op("sp", lambda e, xs_=xs_, src=src: e.dma_start(out=xe[xs_], in_=src), writes=[b_xe[xs_]], dma=True)

                def omm(e, s_=s_, c2=c2, xs_=xs_):
                    for kc in range(32):
                        lw = wo[s_][:, kc, c2 * 128:(c2 + 1) * 128]
                        for (t0, n) in RNG:
                            e.matmul(ps[:, EA + 384 + t0: EA + 384 + t0 + n], lhsT=lw, rhs=mTs[:, kc, t0:t0 + n],
                                     start=(kc == 0), stop=False)
                    last = None
                    for i in range(9):
                        last = e.matmul(ps[:, EA + 384 + i * 128: EA + 384 + (i + 1) * 128], lhsT=xe[xs_][:, i, :], rhs=identf,
                                        start=False, stop=True, skip_group_check=True)
                    return last
                sc.op("pe", omm, reads=[b_wo[s_], b_mTs, b_xe[xs_], b_cf], writes=[b_EA])
                sc.op("act", lambda e, xs_=xs_: e.activation(out=hst[xs_], in_=ps[:, EA + 384:EA + 1536], func=AF.Copy),
                      reads=[b_EA], writes=[b_hst[xs_]])
                sc.op("dve", lambda e, xs_=xs_: e.tensor_tensor(out=sq, in0=hst[xs_], in1=hst[xs_], op=ALU.mult),
                      reads=[b_hst[xs_]], writes=[b_sq])
                sc.op("pe", lambda e, dc=dc: ones_mm(e, EB, sq, dc == 0, dc == 31), reads=[b_sq, b_cf], writes=[b_EB])
                sc.op("sp", lambda e, xs_=xs_, dc=dc: e.dma_start(out=hT_d[dc], in_=hst[xs_]), reads=[b_hst[xs_]], writes=[B("hT%d" % dc)], dma=True)
        sc.op("dve", lambda e: e.tensor_scalar(out=rstdf, in0=ps[:, EB + 384:EB + 1536], scalar1=1.0 / D, scalar2=1e-6, op0=ALU.mult, op1=ALU.add),
              reads=[b_EB], writes=[b_rstdf])
        sc.op("act", lambda e: e.activation(out=rstdf, in_=rstdf, func=AF.Sqrt), reads=[b_rstdf], writes=[b_rstdf])
        sc.op("dve", lambda e: e.reciprocal(out=rstdf, in_=rstdf), reads=[b_rstdf], writes=[b_rstdf])
        sc.barrier()

        sc.enabled = (upto >= 7)
        fT = carve(0, (32, EXT), BF16)
        b_fT = B("fT")
        hl = [carve(72 * KB, (EXT,), F32), carve(77 * KB, (EXT,), F32)]
        b_hl = [B("hl0"), B("hl1")]
        for dc in range(32):
            s_ = dc % 2
            sc.op("sp", lambda e, s_=s_, dc=dc: e.dma_start(out=hl[s_], in_=hT_d[dc]), reads=[B("hT%d" % dc)], writes=[b_hl[s_]], dma=True)
            sc.op("dve", lambda e, s_=s_, dc=dc: e.scalar_tensor_tensor(out=fT[:, dc, :], in0=hl[s_], scalar=gffn[:, dc:dc + 1], in1=rstdf,
                                                                    op0=ALU.mult, op1=ALU.mult),
                  reads=[b_hl[s_], b_rstdf, b_prm], writes=[b_fT])
        sc.op("dve", lambda e: e.tensor_scalar(out=fT[:, :, 0:128], in0=fT[:, :, 0:128], scalar1=flag, scalar2=None, op0=ALU.mult),
              reads=[b_fT, b_prm], writes=[b_fT])
        sc.barrier()

        sc.enabled = (upto >= 8)
        wg = [carve(72 * KB, (32, 256), BF16), carve(88 * KB, (32, 256), BF16)]
        wv = [carve(104 * KB, (32, 256), BF16), carve(120 * KB, (32, 256), BF16)]
        b_wg, b_wv = [B("wg0"), B("wg1")], [B("wv0"), B("wv1")]
        tg = carve(136 * KB, (TOK,), F32)
        tv = carve(140 * KB, (TOK,), F32)
        sgl = carve(144 * KB, (TOK,), F32)
        b_tg, b_tv, b_sgl = B("tg"), B("tv"), B("sgl")
        gst = [carve(148 * KB, (TOK,), BF16), carve(150 * KB, (TOK,), BF16)]
        b_gst = [B("gst0"), B("gst1")]
        O0 = 384 + 128

        def conv3(base, b_acc, dst, b_dst, ch):
            sc.op("act", lambda e: e.activation(out=dst, in_=ps[:, base + O0: base + O0 + TOK], func=AF.Identity,
                                                scale=wffn[:, ch, 2:3], bias=bffn[:, ch:ch + 1]),
                  reads=[b_acc, b_prm], writes=[b_dst])
            sc.op("dve", lambda e: e.scalar_tensor_tensor(out=dst, in0=ps[:, base + O0 - 1: base + O0 - 1 + TOK], scalar=wffn[:, ch, 1:2], in1=dst,
                                                          op0=ALU.mult, op1=ALU.add), reads=[b_acc, b_dst, b_prm], writes=[b_dst])
            sc.op("dve", lambda e: e.scalar_tensor_tensor(out=dst, in0=ps[:, base + O0 - 2: base + O0 - 2 + TOK], scalar=wffn[:, ch, 0:1], in1=dst,
                                                          op0=ALU.mult, op1=ALU.add), reads=[b_acc, b_dst, b_prm], writes=[b_dst])
        for fb in range(43):
            s_ = fb % 2
            wload(wg[s_], w_up_v[:, :, fb * 256:(fb + 1) * 256], b_wg[s_])
            wload(wv[s_], w_up_v[:, :, DFF + fb * 256: DFF + (fb + 1) * 256], b_wv[s_])
            for c2 in range(2):
                fc = fb * 2 + c2
                gs = fc % 2
                sc.op("pe", lambda e, s_=s_, c2=c2: mm_ext(e, EA, lambda kc: wg[s_][:, kc, c2 * 128:(c2 + 1) * 128], fT, 32),
                      reads=[b_wg[s_], b_fT], writes=[b_EA])
                sc.op("pe", lambda e, s_=s_, c2=c2: mm_ext(e, EB, lambda kc: wv[s_][:, kc, c2 * 128:(c2 + 1) * 128], fT, 32),
                      reads=[b_wv[s_], b_fT], writes=[b_EB])
                conv3(EA, b_EA, tg, b_tg, fc)
                conv3(EB, b_EB, tv, b_tv, NFC + fc)
                sc.op("act", lambda e: e.activation(out=sgl, in_=tg, func=AF.Silu), reads=[b_tg], writes=[b_sgl])
                sc.op("dve", lambda e, gs=gs: e.tensor_tensor(out=gst[gs], in0=sgl, in1=tv, op=ALU.mult),
                      reads=[b_sgl, b_tv], writes=[b_gst[gs]])
                sc.op("sp", lambda e, gs=gs, fc=fc: e.dma_start(out=gT_d[fc], in_=gst[gs]), reads=[b_gst[gs]], writes=[B("gT")], dma=True)
        sc.barrier()

        sc.enabled = (upto >= 9)
        gTh = carve(0, (NFC, 512), BF16)
        b_gTh = B("gTh")
        wd = [carve(86 * KB, (NFC, 256), BF16), carve(129 * KB, (NFC, 256), BF16)]
        b_wd = [B("wd0"), B("wd1")]
        hres = [carve(172 * KB, (512,), F32), carve(174 * KB, (512,), F32)]
        b_hres = [B("hres0"), B("hres1")]
        h2st = [carve(176 * KB, (512,), F32), carve(178 * KB, (512,), F32)]
        b_h2st = [B("h2st0"), B("h2st1")]
        sq6 = carve(180 * KB, (512,), F32)
        b_sq6 = B("sq6")
        rstd2 = carve(182 * KB, (512,), F32)
        b_rstd2 = B("rstd2")
        hl6 = [carve(172 * KB, (512,), F32), carve(174 * KB, (512,), F32)]
        b_hl6 = b_hres
        of6 = [carve(176 * KB, (512,), F32), carve(178 * KB, (512,), F32)]
        b_of6 = b_h2st
        ost = carve(0, (4, D), F32)
        b_ost = B("ost")
        dacc = [(0, B("psD0")), (512, B("psD1"))]
        SSB = 1024
        b_ss6 = B("psSS")
        trb = [(1536, B("psTr0")), (2048, B("psTr1")), (2560, B("psTr2")), (3072, B("psTr3"))]
        trcnt = 0
        for half in range(2):
            t0h = half * 512
            sc.op("sp", lambda e, t0h=t0h: e.dma_start(out=gTh, in_=gT_d[:, :, t0h:t0h + 512].rearrange("c p t -> p c t")),
                  reads=[B("gT")], writes=[b_gTh], dma=True)
            for dc in range(32):
                s_ = dc % 2
                ws_ = (dc // 2) % 2
                wc_ = dc % 2
                if wc_ == 0:
                    wload(wd[ws_], w_down_v[:, :, (dc // 2) * 256:(dc // 2 + 1) * 256], b_wd[ws_])
                base, b_acc = dacc[dc % 2]
                sc.op("sp", lambda e, s_=s_, dc=dc, t0h=t0h: e.dma_start(out=hres[s_], in_=hT_d[dc, :, 128 + t0h: 128 + t0h + 512]),
                      reads=[B("hT%d" % dc)], writes=[b_hres[s_]], dma=True)

                def dmm6(e, ws_=ws_, wc_=wc_, base=base):
                    last = None
                    for fc in range(NFC):
                        last = e.matmul(ps[:, base:base + 512], lhsT=wd[ws_][:, fc, wc_ * 128:(wc_ + 1) * 128], rhs=gTh[:, fc, :],
                                        start=(fc == 0), stop=(fc == NFC - 1))
                    return last
                sc.op("pe", dmm6, reads=[b_wd[ws_], b_gTh], writes=[b_acc])
                sc.op("dve", lambda e, s_=s_, base=base: e.tensor_tensor(out=h2st[s_], in0=ps[:, base:base + 512], in1=hres[s_], op=ALU.add),
                      reads=[b_acc, b_hres[s_]], writes=[b_h2st[s_]])
                sc.op("act", lambda e, s_=s_: e.activation(out=sq6, in_=h2st[s_], func=AF.Square), reads=[b_h2st[s_]], writes=[b_sq6])
                sc.op("pe", lambda e, dc=dc: e.matmul(ps[:, SSB:SSB + 512], lhsT=onesf, rhs=sq6, start=(dc == 0), stop=(dc == 31)),
                      reads=[b_sq6, b_cf], writes=[b_ss6])
                sc.op("sp", lambda e, s_=s_, dc=dc, t0h=t0h: e.dma_start(out=h2T_d[dc, :, t0h:t0h + 512], in_=h2st[s_]),
                      reads=[b_h2st[s_]], writes=[B("h2T%d" % dc)], dma=True)
            sc.op("dve", lambda e: e.tensor_scalar(out=rstd2, in0=ps[:, SSB:SSB + 512], scalar1=1.0 / D, scalar2=1e-6, op0=ALU.mult, op1=ALU.add),
                  reads=[b_ss6], writes=[b_rstd2])
            sc.op("act", lambda e: e.activation(out=rstd2, in_=rstd2, func=AF.Sqrt), reads=[b_rstd2], writes=[b_rstd2])
            sc.op("dve", lambda e: e.reciprocal(out=rstd2, in_=rstd2), reads=[b_rstd2], writes=[b_rstd2])
            sc.barrier()
            for dc in range(32):
                s_ = dc % 2
                sc.op("sp", lambda e, s_=s_, dc=dc, t0h=t0h: e.dma_start(out=hl6[s_], in_=h2T_d[dc, :, t0h:t0h + 512]),
                      reads=[B("h2T%d" % dc)], writes=[b_hl6[s_]], dma=True)
                sc.op("dve", lambda e, s_=s_, dc=dc: e.scalar_tensor_tensor(out=of6[s_], in0=hl6[s_], scalar=gfin[:, dc:dc + 1], in1=rstd2,
                                                                        op0=ALU.mult, op1=ALU.mult),
                      reads=[b_hl6[s_], b_rstd2, b_prm], writes=[b_of6[s_]])
                for t in range(4):
                    tb, b_tb = trb[trcnt % 4]
                    trcnt += 1
                    sc.op("pe", lambda e, s_=s_, t=t, tb=tb: e.transpose(ps[:, tb:tb + 128], of6[s_][:, t * 128:(t + 1) * 128], identf),
                          reads=[b_of6[s_], b_cf], writes=[b_tb])
                    if t % 2 == 0:
                        sc.op("act", lambda e, t=t, tb=tb, dc=dc: e.activation(out=ost[:, t, dc * 128:(dc + 1) * 128], in_=ps[:, tb:tb + 128], func=AF.Copy),
                              reads=[b_tb], writes=[b_ost])
                    else:
                        sc.op("dve", lambda e, t=t, tb=tb, dc=dc: e.tensor_copy(out=ost[:, t, dc * 128:(dc + 1) * 128], in_=ps[:, tb:tb + 128]),
                              reads=[b_tb], writes=[b_ost])
            sc.op("sp", lambda e, t0h=t0h: e.dma_start(out=out_d[t0h:t0h + 512, :].rearrange("(t p) n -> p t n", p=128), in_=ost),
                  reads=[b_ost], writes=[B("out")], dma=True)
            sc.barrier()

        sc.finalize()
        sems = {}
        for e_ in ENGS:
            for i in range(NCS):
                sems[("c", e_, i)] = nc.alloc_semaphore("c_%s_%d" % (e_, i))
            if sc.ndma[e_] > 0:
                for i in range(NDS):
                    sems[("d", e_, i)] = nc.alloc_semaphore("d_%s_%d" % (e_, i))

        @block.sync
        def _(e):
            sc.emit("sp", e, sems, final_wait=True)

        @block.gpsimd
        def _(e):
            sc.emit("pool", e, sems)

        @block.tensor
        def _(e):
            sc.emit("pe", e, sems)

        @block.scalar
        def _(e):
            sc.emit("act", e, sems)

        @block.vector
        def _(e):
            sc.emit("dve", e, sems)
    return nc


def _host_consts(c):
    prm_common = None
    attc = np.zeros((128, NATT), np.float32)
    p = np.arange(128, dtype=np.float64)[:, None]
    for G in range(5):
        p0 = E0 if G == 0 else 7168 + 256 * (G - 1)
        kc = np.arange(64, dtype=np.float64)[None, :]
        attc[:, A_DK + G * 64: A_DK + (G + 1) * 64] = np.maximum(p0 - (kc * 128 + p), 0.0)
        n = np.arange(32)[None, :]
        ok = (n < 27 + G) & (n >= (7 - c) * 4)
        attc[:, A_GMASK + G * 32: A_GMASK + (G + 1) * 32] = np.where(ok, 0.0, -1e30)
        attc[:, A_GOK + G * 32: A_GOK + (G + 1) * 32] = np.where(ok, 1.0, 0.0)
    q = np.arange(256, dtype=np.float64)[None, :]
    for kj in range(2):
        dlt = q - (kj * 128 + p)
        attc[:, A_BTV + kj * 256: A_BTV + (kj + 1) * 256] = np.maximum(dlt, 0.0)
        attc[:, A_BTM + kj * 256: A_BTM + (kj + 1) * 256] = np.where(dlt >= 0, 0.0, NEG)
    attc[:, A_QO] = p[:, 0]
    attc[:, A_QO + 1] = p[:, 0] + 128
    kc = np.arange(64, dtype=np.float64)[None, :]
    attc[:, A_DK:A_DK + 64] = E0 - (kc * 128 + p)
    for G in range(5):
        attc[:, A_OWN + G * 32 + 27 + G] = 1.0
    for ti in range(9):
        attc[:, A_QO9 + ti] = p[:, 0] + 128 * ti
    return attc


_NC_CACHE = {}


def kernel(x, g_mix, w_in, w_conv_dw, b_conv_dw, ln_conv_g, ln_conv_b, w_proj_attn, w_proj_conv, w_out,
           g_ffn, w_up, w_ffn_dw, b_ffn_dw, w_down, g_final):
    f32 = np.float32
    x2 = np.asarray(x, f32).reshape(S, D)

    def lay(v, nch):
        return np.ascontiguousarray(np.asarray(v, f32).reshape(nch, 128).T)

    prm = np.zeros((128, NPRM), f32)
    prm[:, P_WCONV:P_WCONV + 496] = np.asarray(w_conv_dw, f32).reshape(31, 16, 128).transpose(2, 1, 0).reshape(128, 496)
    prm[:, P_BCONV:P_BCONV + 16] = lay(b_conv_dw, 16)
    prm[:, P_LNG:P_LNG + 16] = lay(ln_conv_g, 16)
    prm[:, P_LNB:P_LNB + 16] = lay(ln_conv_b, 16)
    prm[:, P_GFFN:P_GFFN + 32] = lay(g_ffn, 32)
    prm[:, P_GFIN:P_GFIN + 32] = lay(g_final, 32)
    prm[:, P_WFFN:P_WFFN + 516] = np.asarray(w_ffn_dw, f32).reshape(3, 172, 128).transpose(2, 1, 0).reshape(128, 516)
    prm[:, P_BFFN:P_BFFN + 172] = lay(b_ffn_dw, 172)
    gbc = np.ascontiguousarray(np.broadcast_to(np.asarray(g_mix, f32)[None, :], (128, D)))
    cf32 = np.concatenate([np.eye(128, dtype=f32), np.ones((128, 128), f32)], axis=1)
    identb = np.eye(128, dtype=f32).astype(ml_dtypes.bfloat16)
    oh2 = np.zeros((128, 32, 128), f32)
    for k in range(64):
        oh2[k, k % 32, :] = 1.0
    oh2 = oh2.reshape(128, 4096).astype(ml_dtypes.bfloat16)
    shared = {
        "w_in": np.ascontiguousarray(w_in, dtype=f32), "w_proj_attn": np.ascontiguousarray(w_proj_attn, dtype=f32),
        "w_proj_conv": np.ascontiguousarray(w_proj_conv, dtype=f32), "w_out": np.ascontiguousarray(w_out, dtype=f32),
        "w_up": np.ascontiguousarray(w_up, dtype=f32), "w_down": np.ascontiguousarray(w_down, dtype=f32),
        "gbc": gbc, "cf32": cf32, "identb": identb, "oh2": oh2,
    }
    in_maps = []
    for c in range(NCORE):
        xr = np.zeros((S, D), f32)
        n = (c + 1) * TOK
        xr[S - n:, :] = x2[:n, :]
        pc = prm.copy()
        pc[:, P_FLAG] = 0.0 if c == 0 else 1.0
        m = dict(shared)
        m["xr"] = xr
        m["prm"] = pc
        m["attc"] = _host_consts(c)
        in_maps.append(m)
    if "nc" not in _NC_CACHE:
        _NC_CACHE["nc"] = build()
    nc = _NC_CACHE["nc"]
    res = run_bass_kernel_spmd(nc, in_maps, core_ids=list(range(NCORE)))
    outs = [np.asarray(res.results[c]["out"], f32) for c in range(NCORE)]
    return np.concatenate(outs, axis=0).reshape(1, S, D)
```

```python
import numpy as np
import ml_dtypes
import concourse.bass as bass
import concourse.mybir as mybir
from concourse.bass_utils import run_bass_kernel_spmd

F32 = mybir.dt.float32
BF16 = mybir.dt.bfloat16
U8 = mybir.dt.uint8
AF = mybir.ActivationFunctionType
ALU = mybir.AluOpType
AX = mybir.AxisListType

D = 4096
S = 8192
NCORE = 8
TOK = 1024
EXT = 1152
E0 = S - EXT
H = 16
NBLK = 32
DFF = 11008
NFC = 86
INW = 18432
NEG = -30000.0
SLOPES = [float(2.0 ** (-8.0 * (h + 1) / 16.0)) for h in range(H)]
QSCALE = float(128 ** -0.5)
RNG = [(0, 128), (128, 512), (640, 512)]
HBG0 = [6, 6, 6, 6, 5, 3, 0, 0]
FB_H = [24] * 8 + [20, 20, 16, 12, 8, 0, 0, 0]
KB = 1024

P_WCONV = 0
P_BCONV = 496
P_LNG = 512
P_LNB = 528
P_GFFN = 544
P_GFIN = 576
P_WFFN = 608
P_BFFN = 1124
P_FLAG = 1296
NPRM = 1304
A_DK = 0
A_GMASK = 320
A_GOK = 480
A_BTV = 640
A_BTM = 1152
A_QO = 1664
A_OWN = 1672
A_QO9 = 1832
NATT = 1848


import os as _os
_KCUT = _os.environ.get("KCUT", "")
_KTRACE = _os.environ.get("KTRACE", "")
_KSKIP = _os.environ.get("KSKIP", "")


class Buf:
    __slots__ = ("name", "w", "r")

    def __init__(self, name):
        self.name = name
        self.w = None
        self.r = []


class Op:
    __slots__ = ("eng", "fn", "deps", "sig", "isdma", "semi", "val")


NDS = 8
NCS = 8
ENGS = ("pe", "act", "dve", "pool", "sp")


class Sched:
    def __init__(self):
        self.ops = {e: [] for e in ENGS}
        self.fence = []
        self.last_dma = {e: {} for e in ENGS}
        self.ndma = {e: 0 for e in ENGS}
        self.enabled = True

    def op(self, eng, fn, reads=(), writes=(), dma=False):
        if not self.enabled:
            return None
        o = Op()
        o.eng = eng
        o.fn = fn
        o.isdma = dma
        o.sig = False
        o.semi = None
        o.val = None
        deps = []
        for b in reads:
            if b.w is not None:
                deps.append(b.w)
        for b in writes:
            if b.w is not None:
                deps.append(b.w)
            deps.extend(b.r)
        deps.extend(self.fence)
        dd = []
        seen = set()
        for d in deps:
            if id(d) in seen:
                continue
            seen.add(id(d))
            if d.eng == eng and eng == "pe" and not d.isdma:
                continue
            dd.append(d)
        if dma:
            k = self.ndma[eng]
            self.ndma[eng] += 1
            slot = k % NDS
            o.semi = ("d", eng, slot)
            o.val = 16 * (k // NDS + 1)
            prev = self.last_dma[eng].get(slot)
            if prev is not None and id(prev) not in seen:
                dd.append(prev)
            self.last_dma[eng][slot] = o
            o.sig = True
        o.deps = dd
        for d in dd:
            d.sig = True
        for b in reads:
            b.r.append(o)
        for b in writes:
            b.w = o
            b.r = []
        self.ops[eng].append(o)
        return o

    def barrier(self):
        if not self.enabled:
            return
        f = []
        for e in ENGS:
            for o in reversed(self.ops[e]):
                if not o.isdma:
                    f.append(o)
                    o.sig = True
                    break
            f.extend(self.last_dma[e].values())
        self.fence = f

    def finalize(self):
        for e in ENGS:
            n = 0
            for o in self.ops[e]:
                if o.isdma or not o.sig:
                    continue
                o.semi = ("c", e, n % NCS)
                o.val = n // NCS + 1
                n += 1

    def emit(self, eng, e, sems, final_wait=False):
        waited = {}
        for o in self.ops[eng]:
            for d in o.deps:
                if waited.get(d.semi, 0) >= d.val:
                    continue
                e.wait_ge(sems[d.semi], d.val)
                waited[d.semi] = d.val
            ins = o.fn(e)
            if o.sig:
                ins.then_inc(sems[o.semi], 16 if o.isdma else 1)
            if _KTRACE:
                print("TR", eng, "L%d" % o.fn.__code__.co_firstlineno, "waits", [(d.semi, d.val) for d in o.deps],
                      "inc", (o.semi, o.val) if o.sig else None)
        if final_wait:
            for o in self.last_dma[eng].values():
                if waited.get(o.semi, 0) >= o.val:
                    continue
                e.wait_ge(sems[o.semi], o.val)
                waited[o.semi] = o.val


def build(upto=99, debug=False):
    nc = bass.Bass("TRN2", target_bir_lowering=False)

    def din(name, shape, dt=F32):
        return nc.dram_tensor(name, list(shape), dt, kind="ExternalInput").ap()

    xr = din("xr", [S, D])
    w_in = din("w_in", [D, INW])
    w_pa = din("w_proj_attn", [2048, D])
    w_pc = din("w_proj_conv", [2048, D])
    w_out = din("w_out", [D, D])
    w_up = din("w_up", [D, 2 * DFF])
    w_down = din("w_down", [DFF, D])
    gbc_d = din("gbc", [128, D])
    prm_d = din("prm", [128, NPRM])
    cf32_d = din("cf32", [128, 256])
    identb_d = din("identb", [128, 128], BF16)
    attc_d = din("attc", [128, NATT])
    oh2_d = din("oh2", [128, 4096], BF16)
    out_d = nc.dram_tensor("out", [TOK, D], F32, kind="ExternalOutput").ap()

    def dscr(name, shape, dt):
        if debug:
            return nc.dram_tensor(name, list(shape), dt, kind="ExternalOutput").ap()
        return nc.dram_tensor(name, list(shape), dt).ap()

    kT_d = dscr("kT_s", [H, 128, S], BF16)
    vT_d = dscr("vT_s", [H, 128, S], BF16)
    qT_d = dscr("qT_s", [H, 128, EXT], BF16)
    yT_d = dscr("yT_s", [16, 128, EXT], F32)
    sgT_d = dscr("sgT_s", [64, 128, EXT], BF16)
    oT_d = dscr("oT_s", [H, 128, EXT], BF16)
    mT_d = dscr("mT_s", [32, 128, EXT], BF16)
    hT_d = dscr("hT_s", [32, 128, EXT], F32)
    gT_d = dscr("gT_s", [NFC, 128, TOK], BF16)
    h2T_d = dscr("h2T_s", [32, 128, TOK], F32)

    w_in_v = w_in.rearrange("(kc p) n -> p kc n", p=128)
    w_pa_v = w_pa.rearrange("(kc p) n -> p kc n", p=128)
    w_pc_v = w_pc.rearrange("(kc p) n -> p kc n", p=128)
    w_out_v = w_out.rearrange("(kc p) n -> p kc n", p=128)
    w_up_v = w_up.rearrange("(kc p) n -> p kc n", p=128)
    w_down_v = w_down.rearrange("(kc p) n -> p kc n", p=128)

    sc = Sched()
    bufs = {}

    def B(name):
        b = bufs.get(name)
        if b is None:
            b = Buf(name)
            bufs[name] = b
        return b

    with (
        nc.sbuf_tensor("arena", [128, 206 * KB], U8) as arena,
        nc.psum_tensor("ps", [128, 4096], F32) as ps,
        nc.Block() as block,
    ):
        def carve(off, shape, dt):
            n = 1
            for s_ in shape:
                n *= s_
            bs = 4 if dt == F32 else 2
            ap = arena[:, off:off + n * bs].bitcast(dt)
            if len(shape) == 2:
                ap = ap.rearrange("p (a b) -> p a b", a=shape[0])
            return ap

        PB = 188 * KB
        prm = carve(PB, (NPRM,), F32)
        cf32 = carve(PB + 5216, (256,), F32)
        identf = cf32[:, 0:128]
        onesf = cf32[:, 128:256]
        identb = carve(PB + 6240, (128,), BF16)
        kms = carve(PB + 6496, (H, NBLK), F32)
        smalls = carve(PB + 8544, (64,), F32)
        ss_t = [smalls[:, 0:1], smalls[:, 1:2]]
        rs_t = [smalls[:, 2:3], smalls[:, 3:4]]
        wconv = prm[:, P_WCONV:P_WCONV + 496].rearrange("p (c k) -> p c k", k=31)
        bconv = prm[:, P_BCONV:P_BCONV + 16]
        lng = prm[:, P_LNG:P_LNG + 16]
        lnb = prm[:, P_LNB:P_LNB + 16]
        gffn = prm[:, P_GFFN:P_GFFN + 32]
        gfin = prm[:, P_GFIN:P_GFIN + 32]
        wffn = prm[:, P_WFFN:P_WFFN + 516].rearrange("p (c k) -> p c k", k=3)
        bffn = prm[:, P_BFFN:P_BFFN + 172]
        flag = prm[:, P_FLAG:P_FLAG + 1]

        b_prm, b_cf, b_idb = B("prm"), B("cf32"), B("identb")
        sc.op("sp", lambda e: e.dma_start(out=prm, in_=prm_d), writes=[b_prm], dma=True)
        sc.op("sp", lambda e: e.dma_start(out=cf32, in_=cf32_d), writes=[b_cf], dma=True)
        sc.op("sp", lambda e: e.dma_start(out=identb, in_=identb_d), writes=[b_idb], dma=True)

        EA, EB = 0, 1536
        b_EA, b_EB = B("psEA"), B("psEB")

        def mm_ext(e, base, lhs_fn, rhs3, nk):
            last = None
            for kc in range(nk):
                lw = lhs_fn(kc)
                for (t0, n) in RNG:
                    last = e.matmul(ps[:, base + 384 + t0: base + 384 + t0 + n], lhsT=lw,
                                    rhs=rhs3[:, kc, t0:t0 + n], start=(kc == 0), stop=(kc == nk - 1))
            return last

        def wload(dst, src, bdst):
            return sc.op("pool", lambda e: e.dma_start(out=dst, in_=src), writes=[bdst], dma=True)

        def make_uT(row0, ntiles, uT, b_uT, gbc, b_gbc, xts, xss, tag):
            b_xt = [B(tag + "xt0"), B(tag + "xt1")]
            b_xs = [B(tag + "xs0"), B(tag + "xs1")]
            b_ss = [B("ss0"), B("ss1")]
            b_rs = [B("rs0"), B("rs1")]
            b_tp = [B("psb6"), B("psb7")]
            tpv = [ps[:, 3072:3584].bitcast(BF16).rearrange("p (j t) -> p j t", t=128),
                   ps[:, 3584:4096].bitcast(BF16).rearrange("p (j t) -> p j t", t=128)]
            cnt = 0
            for i in range(ntiles):
                s_ = i % 2
                xt, xs, ss, rs = xts[s_], xss[s_], ss_t[s_], rs_t[s_]
                src = xr[row0 + i * 128: row0 + (i + 1) * 128, :]
                sc.op("sp", lambda e, xt=xt, src=src: e.dma_start(out=xt, in_=src), writes=[b_xt[s_]], dma=True)
                sc.op("dve", lambda e, ss=ss: e.memset(ss, 0.0), writes=[b_ss[s_]])
                sc.op("act", lambda e, xt=xt, xs=xs, ss=ss: e.activation(out=xs, in_=xt, func=AF.Square, accum_out=ss),
                      reads=[b_xt[s_]], writes=[b_xs[s_], b_ss[s_]])
                sc.op("dve", lambda e, ss=ss, rs=rs: e.tensor_scalar(out=rs, in0=ss, scalar1=1.0 / D, scalar2=1e-6,
                                                                      op0=ALU.mult, op1=ALU.add),
                      reads=[b_ss[s_]], writes=[b_rs[s_]])
                sc.op("act", lambda e, rs=rs: e.activation(out=rs, in_=rs, func=AF.Sqrt), reads=[b_rs[s_]], writes=[b_rs[s_]])
                sc.op("dve", lambda e, rs=rs: e.reciprocal(out=rs, in_=rs), reads=[b_rs[s_]], writes=[b_rs[s_]])
                sc.op("dve", lambda e, xt=xt, xs=xs, rs=rs: e.scalar_tensor_tensor(
                    out=xs, in0=xt, scalar=rs, in1=gbc, op0=ALU.mult, op1=ALU.mult),
                    reads=[b_xt[s_], b_rs[s_], b_gbc], writes=[b_xs[s_]])
                for q4 in range(4):
                    pb = cnt % 2
                    cnt += 1

                    def tr(e, xs=xs, q4=q4, pb=pb):
                        last = None
                        for j in range(8):
                            kc = q4 * 8 + j
                            last = e.transpose(tpv[pb][:, j, :], xs[:, kc * 128:(kc + 1) * 128], identb)
                        return last
                    sc.op("pe", tr, reads=[b_xs[s_], b_idb], writes=[b_tp[pb]])
                    dst = uT[:, q4 * 8:(q4 + 1) * 8, i * 128:(i + 1) * 128]
                    if q4 % 2 == 0:
                        sc.op("act", lambda e, dst=dst, pb=pb: e.activation(out=dst, in_=tpv[pb], func=AF.Copy),
                              reads=[b_tp[pb]], writes=[b_uT])
                    else:
                        sc.op("dve", lambda e, dst=dst, pb=pb: e.tensor_copy(out=dst, in_=tpv[pb]),
                              reads=[b_tp[pb]], writes=[b_uT])

        sc.enabled = (upto >= 1)
        uT1 = carve(0, (32, 1024), BF16)
        gbc = carve(64 * KB, (D,), F32)
        xts = [carve(80 * KB, (D,), F32), carve(96 * KB, (D,), F32)]
        xss = [carve(112 * KB, (D,), BF16), carve(120 * KB, (D,), BF16)]
        wb = [carve((128 + 16 * i) * KB, (32, 256), BF16) for i in range(3)]
        b_wb = [B("p1wb%d" % i) for i in range(3)]
        kst = [carve(176 * KB, (1024,), BF16), carve(178 * KB, (1024,), BF16)]
        b_kst = [B("kst0"), B("kst1")]
        vst = [carve(180 * KB, (8, 256), BF16), carve(184 * KB, (8, 256), BF16)]
        b_vst = [B("vst0"), B("vst1")]
        b_uT1, b_gbc, b_kms = B("uT1"), B("gbc"), B("kms")
        ubf = carve(197 * KB, (32, NBLK), F32)
        ubhl = carve(201 * KB, (32, 64), BF16)
        ubt = carve(PB + 8800, (64,), F32)[:, 0:32]
        b_ubf, b_ubhl, b_ubt = B("ubf"), B("ubhl"), B("ubt")
        sc.op("sp", lambda e: e.dma_start(out=gbc, in_=gbc_d), writes=[b_gbc], dma=True)
        kacc = [(0, B("psK0")), (1024, B("psK1"))]
        vacc = [(2048, B("psV0")), (2560, B("psV1"))]
        wcnt = 0
        kcnt = 0
        vcnt = 0
        for g in range(8):
            make_uT(g * 1024, 8, uT1, b_uT1, gbc, b_gbc, xts, xss, "p1")
            if _KCUT == "a":
                sc.enabled = False
            sc.op("dve", lambda e, g=g: e.tensor_reduce(out=ubf[:, :, g * 4:(g + 1) * 4],
                                                     in_=uT1.rearrange("p k (b t) -> p k b t", t=256), axis=AX.X, op=ALU.add),
                  reads=[b_uT1], writes=[b_ubf])
            if g == 7:
                sc.op("dve", lambda e: e.tensor_scalar(out=ubf, in0=ubf, scalar1=1.0 / 256.0, scalar2=None, op0=ALU.mult),
                      reads=[b_ubf], writes=[b_ubf])
                sc.op("dve", lambda e: e.tensor_copy(out=ubhl[:, :, 0:32], in_=ubf), reads=[b_ubf], writes=[b_ubhl])
                sc.op("dve", lambda e: e.tensor_tensor(out=ubf, in0=ubf, in1=ubhl[:, :, 0:32], op=ALU.subtract),
                      reads=[b_ubf, b_ubhl], writes=[b_ubf])
                sc.op("dve", lambda e: e.tensor_copy(out=ubhl[:, :, 32:64], in_=ubf), reads=[b_ubf], writes=[b_ubhl])
            for hb16 in range(16):
                hb = hb16 % 8
                isv = hb16 >= 8
                if g < HBG0[hb]:
                    continue
                ws = wcnt % 3
                wcnt += 1
                wload(wb[ws], w_in_v[:, :, 2048 + hb16 * 256: 2048 + (hb16 + 1) * 256], b_wb[ws])
                for hh in range(2):
                    h = hb * 2 + hh
                    base, b_acc = kacc[kcnt % 2]
                    ks = kcnt % 2
                    kcnt += 1

                    def kmm(e, ws=ws, hh=hh, base=base):
                        last = None
                        for kc in range(32):
                            lw = wb[ws][:, kc, hh * 128:(hh + 1) * 128]
                            for half in range(2):
                                last = e.matmul(ps[:, base + half * 512: base + (half + 1) * 512], lhsT=lw,
                                                rhs=uT1[:, kc, half * 512:(half + 1) * 512],
                                                start=(kc == 0), stop=(kc == 31))
                        return last
                    sc.op("pe", kmm, reads=[b_wb[ws], b_uT1], writes=[b_acc])
                    if "c" not in _KSKIP:
                        sc.op("act", lambda e, ks=ks, base=base: e.activation(out=kst[ks], in_=ps[:, base:base + 1024], func=AF.Copy),
                              reads=[b_acc], writes=[b_kst[ks]])
                    if g == 7 and not isv:
                        def kmean_mm(e, ws=ws, hh=hh):
                            last = None
                            for kc in range(32):
                                last = e.matmul(ps[:, 2560:2624], lhsT=wb[ws][:, kc, hh * 128:(hh + 1) * 128], rhs=ubhl[:, kc, :],
                                                start=(kc == 0), stop=(kc == 31))
                            return last
                        sc.op("pe", kmean_mm, reads=[b_wb[ws], b_ubhl], writes=[B("psV1")])
                        sc.op("act", lambda e: e.activation(out=ubt, in_=ps[:, 2592:2624], func=AF.Copy), reads=[B("psV1")], writes=[b_ubt])
                        sc.op("dve", lambda e, h=h: e.tensor_tensor(out=kms[:, h, :], in0=ps[:, 2560:2592], in1=ubt, op=ALU.add),
                              reads=[B("psV1"), b_ubt], writes=[b_kms])
                    if isv:
                        sc.op("sp", lambda e, ks=ks, h=h, g=g: e.dma_start(out=vT_d[h, :, g * 1024:(g + 1) * 1024], in_=kst[ks]),
                              reads=[b_kst[ks]], writes=[B("vT%d" % h)], dma=True)
                    else:
                        sc.op("sp", lambda e, ks=ks, h=h, g=g: e.dma_start(out=kT_d[h, :, g * 1024:(g + 1) * 1024], in_=kst[ks]),
                              reads=[b_kst[ks]], writes=[B("kT%d" % h)], dma=True)
            if _KCUT == "b":
                sc.enabled = False
            if _KCUT == "c":
                sc.enabled = False
        sc.barrier()

        sc.enabled = (upto >= 2)
        uT2 = carve(0, (32, EXT), BF16)
        b_uT2 = B("uT2")
        gbc2 = carve(72 * KB, (D,), F32)
        b_gbc2 = B("gbc2")
        sc.op("sp", lambda e: e.dma_start(out=gbc2, in_=gbc_d), writes=[b_gbc2], dma=True)
        xts2 = [carve(88 * KB, (D,), F32), carve(104 * KB, (D,), F32)]
        xss2 = [carve(120 * KB, (D,), BF16), carve(128 * KB, (D,), BF16)]
        make_uT(E0, 9, uT2, b_uT2, gbc2, b_gbc2, xts2, xss2, "p2")
        sc.barrier()
        wb2 = [carve((72 + 16 * i) * KB, (32, 256), BF16) for i in range(4)]
        b_wb2 = [B("p2wb%d" % i) for i in range(4)]
        qst = [carve(136 * KB, (EXT,), BF16), carve(139 * KB, (EXT,), BF16)]
        b_qst = [B("qst0"), B("qst1")]
        sgb = carve(142 * KB, (EXT,), F32)
        b_sgb = B("sgb")
        cpad = [carve(147 * KB, (EXT + 30,), F32), carve(152 * KB, (EXT + 30,), F32)]
        b_cpad = [B("cpad0"), B("cpad1")]
        yb = [carve(157 * KB, (EXT,), F32), carve(162 * KB, (EXT,), F32)]
        b_yb = [B("yb0"), B("yb1")]
        sgst = [carve(167 * KB, (EXT,), BF16), carve(170 * KB, (EXT,), BF16)]
        b_sgst = [B("sgst0"), B("sgst1")]
        for i in range(2):
            sc.op("dve", lambda e, i=i: e.memset(cpad[i][:, 0:30], 0.0), writes=[b_cpad[i]])
        accs = [(EA, b_EA), (EB, b_EB)]
        acnt = 0
        wcnt = 0
        for hb in range(8):
            ws = wcnt % 4
            wcnt += 1
            wload(wb2[ws], w_in_v[:, :, hb * 256:(hb + 1) * 256], b_wb2[ws])
            for hh in range(2):
                h = hb * 2 + hh
                base, b_acc = accs[acnt % 2]
                acnt += 1
                qs = h % 2
                sc.op("pe", lambda e, ws=ws, hh=hh, base=base: mm_ext(
                    e, base, lambda kc: wb2[ws][:, kc, hh * 128:(hh + 1) * 128], uT2, 32),
                    reads=[b_wb2[ws], b_uT2], writes=[b_acc])
                sc.op("act", lambda e, qs=qs, base=base: e.activation(
                    out=qst[qs], in_=ps[:, base + 384: base + 1536], func=AF.Copy, scale=QSCALE),
                    reads=[b_acc], writes=[b_qst[qs]])
                sc.op("sp", lambda e, qs=qs, h=h: e.dma_start(out=qT_d[h], in_=qst[qs]),
                      reads=[b_qst[qs]], writes=[B("qT%d" % h)], dma=True)
        for cb in range(8):
            wsa = wcnt % 4
            wcnt += 1
            wload(wb2[wsa], w_in_v[:, :, 6144 + cb * 256: 6144 + (cb + 1) * 256], b_wb2[wsa])
            wsb = wcnt % 4
            wcnt += 1
            wload(wb2[wsb], w_in_v[:, :, 8192 + cb * 256: 8192 + (cb + 1) * 256], b_wb2[wsb])
            for c2 in range(2):
                cc = cb * 2 + c2
                cs = cc % 2
                sc.op("pe", lambda e, wsb=wsb, c2=c2: mm_ext(
                    e, EB, lambda kc: wb2[wsb][:, kc, c2 * 128:(c2 + 1) * 128], uT2, 32),
                    reads=[b_wb2[wsb], b_uT2], writes=[b_EB])
                sc.op("act", lambda e: e.activation(out=sgb, in_=ps[:, EB + 384:EB + 1536], func=AF.Sigmoid),
                      reads=[b_EB], writes=[b_sgb])
                sc.op("pe", lambda e, wsa=wsa, c2=c2: mm_ext(
                    e, EA, lambda kc: wb2[wsa][:, kc, c2 * 128:(c2 + 1) * 128], uT2, 32),
                    reads=[b_wb2[wsa], b_uT2], writes=[b_EA])
                sc.op("dve", lambda e, cs=cs: e.tensor_tensor(out=cpad[cs][:, 30:30 + EXT], in0=ps[:, EA + 384:EA + 1536],
                                                              in1=sgb, op=ALU.mult),
                      reads=[b_EA, b_sgb], writes=[b_cpad[cs]])
                sc.op("act", lambda e, cs=cs, cc=cc: e.activation(
                    out=yb[cs], in_=cpad[cs][:, 0:EXT], func=AF.Identity,
                    scale=wconv[:, cc, 0:1], bias=bconv[:, cc:cc + 1]),
                    reads=[b_cpad[cs], b_prm], writes=[b_yb[cs]])
                for k in range(1, 31):
                    sc.op("dve", lambda e, cs=cs, cc=cc, k=k: e.scalar_tensor_tensor(
                        out=yb[cs], in0=cpad[cs][:, k:k + EXT], scalar=wconv[:, cc, k:k + 1], in1=yb[cs],
                        op0=ALU.mult, op1=ALU.add), reads=[b_cpad[cs], b_yb[cs]], writes=[b_yb[cs]])
                sc.op("sp", lambda e, cs=cs, cc=cc: e.dma_start(out=yT_d[cc], in_=yb[cs]),
                      reads=[b_yb[cs]], writes=[B("yT%d" % cc)], dma=True)
        for gb in range(32):
            ws = wcnt % 4
            wcnt += 1
            wload(wb2[ws], w_in_v[:, :, 10240 + gb * 256: 10240 + (gb + 1) * 256], b_wb2[ws])
            for c2 in range(2):
                gc = gb * 2 + c2
                base, b_acc = accs[acnt % 2]
                acnt += 1
                gs = gc % 2
                sc.op("pe", lambda e, ws=ws, c2=c2, base=base: mm_ext(
                    e, base, lambda kc: wb2[ws][:, kc, c2 * 128:(c2 + 1) * 128], uT2, 32),
                    reads=[b_wb2[ws], b_uT2], writes=[b_acc])
                sc.op("act", lambda e, gs=gs, base=base: e.activation(
                    out=sgst[gs], in_=ps[:, base + 384: base + 1536], func=AF.Sigmoid),
                    reads=[b_acc], writes=[b_sgst[gs]])
                sc.op("sp", lambda e, gs=gs, gc=gc: e.dma_start(out=sgT_d[gc], in_=sgst[gs]),
                      reads=[b_sgst[gs]], writes=[B("sgT%d" % gc)], dma=True)
        sc.barrier()

        sc.enabled = (upto >= 3)
        kTh = [carve(0, (S,), BF16), carve(16 * KB, (S,), BF16)]
        Va = [carve(32 * KB, (64, 129), BF16), carve(49 * KB, (64, 129), BF16)]
        qTh = [carve(66 * KB, (EXT,), BF16), carve(69 * KB, (EXT,), BF16)]
        oTh = [carve(72 * KB, (EXT,), BF16), carve(75 * KB, (EXT,), BF16)]
        attc = carve(80 * KB, (NATT,), F32)
        oh2 = carve(88 * KB, (32, 128), BF16)
        biask = [carve(96 * KB, (64,), F32), carve(96 * KB + 256, (64,), F32)]
        aqh = [carve(97 * KB, (9,), F32), carve(97 * KB + 64, (9,), F32)]
        causb = carve(98 * KB, (2, 256), BF16)
        onesb = carve(99 * KB, (128,), BF16)
        kmhi = carve(105 * KB, (H, NBLK), BF16)
        kmlo = carve(106 * KB, (H, NBLK), BF16)
        kmf = carve(107 * KB, (H, NBLK), F32)
        tmp = carve(109 * KB, (768,), F32)
        m8 = [tmp[:, 8:16], tmp[:, 16:24]]
        gm = [tmp[:, 32:64], tmp[:, 64:96]]
        incl = [tmp[:, 96:128], tmp[:, 128:160]]
        row = [tmp[:, 160:192], tmp[:, 192:224]]
        dtm = [tmp[:, 224:256], tmp[:, 256:288]]
        rhl = [carve(112 * KB, (64,), BF16), carve(112 * KB + 128, (64,), BF16)]
        coefT = [carve(113 * KB, (EXT,), BF16), carve(116 * KB, (EXT,), BF16)]
        pT = [carve(120 * KB, (640,), BF16), carve(122 * KB, (640,), BF16), carve(124 * KB, (640,), BF16)]
        rdn = [carve(126 * KB, (640,), F32), carve(129 * KB, (640,), F32)]
        vTh = [carve(132 * KB, (S,), BF16), carve(148 * KB, (S,), BF16)]
        b_vTh = [B("vTh0"), B("vTh1")]
        dk = attc[:, A_DK:A_DK + 64]
        gmask = attc[:, A_GMASK:A_GMASK + 160]
        gok = attc[:, A_GOK:A_GOK + 160]
        btm = attc[:, A_BTM:A_BTM + 512].rearrange("p (a b) -> p a b", a=2)
        own01 = attc[:, A_OWN:A_OWN + 160]
        qo9 = attc[:, A_QO9:A_QO9 + 9]
        b_attc, b_oh2 = B("attc"), B("oh2")
        sc.op("sp", lambda e: e.dma_start(out=attc, in_=attc_d), writes=[b_attc], dma=True)
        sc.op("sp", lambda e: e.dma_start(out=oh2, in_=oh2_d.rearrange("p (a b) -> p a b", a=32)), writes=[b_oh2], dma=True)
        b_kTh = [B("kTh0"), B("kTh1")]
        b_Va = [B("Va0"), B("Va1")]
        b_qTh = [B("qTh0"), B("qTh1")]
        b_oTh = [B("oTh0"), B("oTh1")]
        b_pT = [B("pT%d" % i) for i in range(3)]
        b_rdn = [B("rdn0"), B("rdn1")]
        bk = [B("psbank%d" % i) for i in range(8)]
        b_km, b_cau = B("kmhl"), B("causb")
        sc.op("dve", lambda e: e.tensor_copy(out=causb, in_=btm), reads=[b_attc], writes=[b_cau])
        sc.op("dve", lambda e: e.memset(onesb, 1.0), writes=[b_cau])
        sc.op("dve", lambda e: e.tensor_copy(out=kmf, in_=kms), reads=[b_kms], writes=[b_km])
        sc.op("dve", lambda e: e.tensor_copy(out=kmhi, in_=kmf), reads=[b_km], writes=[b_km])
        sc.op("dve", lambda e: e.tensor_tensor(out=kmf, in0=kmf, in1=kmhi, op=ALU.subtract), reads=[b_km], writes=[b_km])
        sc.op("dve", lambda e: e.tensor_copy(out=kmlo, in_=kmf), reads=[b_km], writes=[b_km])
        psg = ps[:, 3072:3104]
        pst = ps[:, 3584:4096].bitcast(BF16)[0:64, 0:128]

        def head_loads(h):
            s_ = h % 2
            fb = FB_H[h]
            nck = 64 - 2 * fb
            sc.op("sp", lambda e: e.dma_start(out=kTh[s_][:, 0:S - fb * 256], in_=kT_d[h, :, fb * 256:S]),
                  reads=[B("kT%d" % h)], writes=[b_kTh[s_]], dma=True)
            sc.op("sp", lambda e: e.dma_start(out=vTh[s_][:, 0:S - fb * 256], in_=vT_d[h, :, fb * 256:S]),
                  reads=[B("vT%d" % h)], writes=[b_vTh[s_]], dma=True)
            sc.op("sp", lambda e: e.dma_start(out=qTh[s_], in_=qT_d[h]), reads=[B("qT%d" % h)], writes=[b_qTh[s_]], dma=True)

        trv = [ps[:, i * 512:(i + 1) * 512].bitcast(BF16).rearrange("p (j t) -> p j t", t=128) for i in range(4)]
        vtc = [0]

        def v_transposes(h):
            s_ = h % 2
            fb = FB_H[h]
            nck = 64 - 2 * fb
            for c8 in range(0, nck, 8):
                bi = vtc[0] % 4
                vtc[0] += 1

                def vtr(e, c8=c8, bi=bi):
                    last = None
                    for j in range(8):
                        last = e.transpose(trv[bi][:, j, :], vTh[s_][:, (c8 + j) * 128:(c8 + j + 1) * 128], identb)
                    return last
                sc.op("pe", vtr, reads=[b_vTh[s_], b_idb], writes=[bk[bi]])
                sc.op("dve", lambda e, c8=c8, bi=bi: e.tensor_copy(out=Va[s_][:, c8:c8 + 8, 0:128], in_=trv[bi]),
                      reads=[bk[bi]], writes=[b_Va[s_]])

        PASSES = [
            dict(t0=0, n=640, rng=[(0, 128), (128, 512)], sb=[0, 1024], off=384, ob=2048, db=3072,
                 sbk=[[0, 1], [2, 3]], obk=[4, 5], dbk=[6, 7], kend=60, groups=[(0, 0, 128, 128), (1, 128, 256, 0), (2, 384, 256, 0)]),
            dict(t0=640, n=512, rng=[(640, 512)], sb=[0, 512], off=-640, ob=1024, db=1536,
                 sbk=[[0], [1]], obk=[2], dbk=[3], kend=64, groups=[(3, 640, 256, 0), (4, 896, 256, 0)]),
        ]
        head_loads(0)
        tcnt = 0
        pcnt = 0
        rcnt = 0
        for h in range(H):
            hs = h % 2
            sl = SLOPES[h]
            if h + 1 < H:
                head_loads(h + 1)
            b_hc = B("hc%d" % hs)
            sc.op("dve", lambda e, hs=hs, sl=sl: e.tensor_scalar(out=biask[hs], in0=dk, scalar1=-sl, scalar2=None, op0=ALU.mult),
                  reads=[b_attc], writes=[b_hc])
            sc.op("dve", lambda e, hs=hs, sl=sl: e.tensor_scalar(out=aqh[hs], in0=qo9, scalar1=-sl, scalar2=None, op0=ALU.mult),
                  reads=[b_attc], writes=[b_hc])
            v_transposes(h)
            b_coef = B("coefT%d" % hs)
            for ti in range(9):
                G = 0 if ti == 0 else (ti - 1) // 2 + 1
                ts = tcnt % 2
                tcnt += 1
                b_t = B("gt%d" % ts)
                tc0 = ti * 128

                def gmm(e, hs=hs, h=h, tc0=tc0):
                    e.matmul(psg, lhsT=qTh[hs][:, tc0:tc0 + 128], rhs=kmhi[:, h, :], start=True, stop=False)
                    return e.matmul(psg, lhsT=qTh[hs][:, tc0:tc0 + 128], rhs=kmlo[:, h, :], start=False, stop=True)
                sc.op("pe", gmm, reads=[b_qTh[hs], b_km], writes=[bk[6]])
                sc.op("dve", lambda e, ts=ts, G=G: e.tensor_tensor(out=gm[ts], in0=psg, in1=gmask[:, G * 32:(G + 1) * 32], op=ALU.add),
                      reads=[bk[6], b_attc], writes=[b_t])
                sc.op("dve", lambda e, ts=ts: e.max(out=m8[ts], in_=gm[ts]), reads=[b_t], writes=[b_t])
                sc.op("dve", lambda e, ts=ts: e.tensor_scalar(out=incl[ts], in0=gm[ts], scalar1=m8[ts][:, 2:3], scalar2=None, op0=ALU.is_ge),
                      reads=[b_t], writes=[b_t])
                sc.op("dve", lambda e, ts=ts, G=G: e.tensor_tensor(out=incl[ts], in0=incl[ts], in1=gok[:, G * 32:(G + 1) * 32], op=ALU.mult),
                      reads=[b_t, b_attc], writes=[b_t])
                sc.op("dve", lambda e, ts=ts, G=G: e.tensor_tensor(out=incl[ts], in0=incl[ts], in1=own01[:, G * 32:(G + 1) * 32], op=ALU.add),
                      reads=[b_t, b_attc], writes=[b_t])
                sc.op("dve", lambda e, ts=ts: e.tensor_scalar(out=row[ts], in0=incl[ts], scalar1=-1.0, scalar2=-NEG, op0=ALU.add, op1=ALU.mult),
                      reads=[b_t], writes=[b_t])
                sc.op("dve", lambda e, ts=ts, hs=hs, ti=ti: e.tensor_scalar(out=row[ts], in0=row[ts], scalar1=aqh[hs][:, ti:ti + 1], scalar2=None, op0=ALU.add),
                      reads=[b_t, b_hc], writes=[b_t])
                sc.op("dve", lambda e, ts=ts: e.tensor_copy(out=rhl[ts][:, 0:32], in_=row[ts]), reads=[b_t], writes=[b_t])
                sc.op("dve", lambda e, ts=ts: e.tensor_tensor(out=dtm[ts], in0=row[ts], in1=rhl[ts][:, 0:32], op=ALU.subtract),
                      reads=[b_t], writes=[b_t])
                sc.op("dve", lambda e, ts=ts: e.tensor_copy(out=rhl[ts][:, 32:64], in_=dtm[ts]), reads=[b_t], writes=[b_t])
                sc.op("pe", lambda e, ts=ts: e.transpose(pst, rhl[ts], identb), reads=[b_t, b_idb], writes=[bk[7]])
                sc.op("act", lambda e, hs=hs, tc0=tc0: e.activation(out=coefT[hs][0:64, tc0:tc0 + 128], in_=pst, func=AF.Copy),
                      reads=[bk[7]], writes=[b_coef])

            def attn_pass(P, h=h, hs=hs, b_hc=b_hc, b_coef=b_coef):
                nonlocal pcnt, rcnt
                fb = FB_H[h]
                kcs = list(range(2 * fb, P["kend"]))
                nk = len(kcs)
                t0, n, off = P["t0"], P["n"], P["off"]
                slot_of = {}

                def emit_S(i):
                    kc = kcs[i]
                    sl_ = i % 2
                    slot_of[i] = sl_
                    sb = P["sb"][sl_]
                    nblk = kc // 2
                    kk = (kc - 2 * fb) * 128
                    diag = [g_ for g_ in P["groups"] if 27 + g_[0] == nblk]

                    def smm(e):
                        last = None
                        for (r0, rn) in P["rng"]:
                            c_ = sb + off + r0
                            e.matmul(ps[:, c_:c_ + rn], lhsT=kTh[hs][:, kk:kk + 128], rhs=qTh[hs][:, r0:r0 + rn], start=True, stop=False)
                        for (r0, rn) in P["rng"]:
                            c_ = sb + off + r0
                            last = e.matmul(ps[:, c_:c_ + rn], lhsT=oh2[0:64, nblk, :], rhs=coefT[hs][0:64, r0:r0 + rn], start=False, stop=True)
                        for (G_, gc0, gW, gb0) in diag:
                            c_ = sb + off + gc0
                            last = e.matmul(ps[:, c_:c_ + gW], lhsT=identb, rhs=causb[:, kc % 2, gb0:gb0 + gW], start=False, stop=True,
                                            skip_group_check=True)
                        return last
                    sbufs = [bk[b_] for b_ in P["sbk"][sl_]]
                    sc.op("pe", smm, reads=[b_kTh[hs], b_qTh[hs], b_oh2, b_coef, b_idb, b_cau], writes=sbufs)
                    pi = pcnt_box[0] % 3
                    pcnt_box[0] += 1
                    slot_of[("p", i)] = pi
                    c_ = sb + off + t0
                    sc.op("act", lambda e, pi=pi, c_=c_, kc=kc: e.activation(out=pT[pi][:, 0:n], in_=ps[:, c_:c_ + n], func=AF.Exp,
                                                                          bias=biask[hs][:, kc:kc + 1]),
                          reads=sbufs + [b_hc], writes=[b_pT[pi]])

                def emit_PV(i):
                    kc = kcs[i]
                    pi = slot_of[("p", i)]
                    vi = kc - 2 * fb

                    def pvm(e):
                        last = None
                        for (r0, rn) in P["rng"]:
                            c_ = P["ob"] + off + r0
                            e.matmul(ps[:, c_:c_ + rn], lhsT=Va[hs][:, vi, 0:128], rhs=pT[pi][:, r0 - t0:r0 - t0 + rn],
                                     start=(i == 0), stop=(i == nk - 1))
                        for (r0, rn) in P["rng"]:
                            c_ = P["db"] + off + r0
                            last = e.matmul(ps[:, c_:c_ + rn], lhsT=onesb, rhs=pT[pi][:, r0 - t0:r0 - t0 + rn],
                                            start=(i == 0), stop=(i == nk - 1))
                        return last
                    sc.op("pe", pvm, reads=[b_pT[pi], b_Va[hs], b_cau], writes=[bk[b_] for b_ in P["obk"] + P["dbk"]])

                emit_S(0)
                for i in range(nk):
                    if i + 1 < nk:
                        emit_S(i + 1)
                    emit_PV(i)
                rs_ = rcnt % 2
                rcnt += 1
                dcol = P["db"] + off + t0
                ocol = P["ob"] + off + t0
                sc.op("dve", lambda e, rs_=rs_, dcol=dcol: e.reciprocal(out=rdn[rs_][:, 0:n], in_=ps[:, dcol:dcol + n]),
                      reads=[bk[b_] for b_ in P["dbk"]], writes=[b_rdn[rs_]])
                sc.op("dve", lambda e, rs_=rs_, ocol=ocol: e.tensor_tensor(out=oTh[hs][:, t0:t0 + n], in0=ps[:, ocol:ocol + n], in1=rdn[rs_][:, 0:n], op=ALU.mult),
                      reads=[bk[b_] for b_ in P["obk"]] + [b_rdn[rs_]], writes=[b_oTh[hs]])

            pcnt_box = [pcnt]
            for P in PASSES:
                attn_pass(P)
            pcnt = pcnt_box[0]
            sc.op("sp", lambda e, hs=hs, h=h: e.dma_start(out=oT_d[h], in_=oTh[hs]), reads=[b_oTh[hs]], writes=[B("oT")], dma=True)
        sc.barrier()

        sc.enabled = (upto >= 4)
        ySB = carve(0, (16, EXT), F32)
        zT = carve(72 * KB, (16, EXT), BF16)
        mean = carve(108 * KB, (EXT,), F32)
        rstd = carve(113 * KB, (EXT,), F32)
        tln = carve(118 * KB, (EXT,), F32)
        ysq = carve(123 * KB, (EXT,), F32)
        b_ySB, b_zT, b_mean, b_rstd, b_tln, b_ysq = B("ySB"), B("zT"), B("mean"), B("rstd"), B("tln"), B("ysq")
        sc.op("sp", lambda e: e.dma_start(out=ySB, in_=yT_d.rearrange("c p t -> p c t")),
              reads=[B("yT%d" % c) for c in range(16)], writes=[b_ySB], dma=True)

        def ones_mm(e, base, src, first, last_):
            last = None
            for (t0, n) in RNG:
                last = e.matmul(ps[:, base + 384 + t0: base + 384 + t0 + n], lhsT=onesf, rhs=src[:, t0:t0 + n],
                                start=first, stop=last_)
            return last
        for cc in range(16):
            sc.op("pe", lambda e, cc=cc: ones_mm(e, EA, ySB[:, cc, :], cc == 0, cc == 15), reads=[b_ySB, b_cf], writes=[b_EA])
            sc.op("act", lambda e, cc=cc: e.activation(out=ysq, in_=ySB[:, cc, :], func=AF.Square), reads=[b_ySB], writes=[b_ysq])
            sc.op("pe", lambda e, cc=cc: ones_mm(e, EB, ysq, cc == 0, cc == 15), reads=[b_ysq, b_cf], writes=[b_EB])
        sc.op("dve", lambda e: e.tensor_scalar(out=mean, in0=ps[:, EA + 384:EA + 1536], scalar1=1.0 / 2048, scalar2=None, op0=ALU.mult),
              reads=[b_EA], writes=[b_mean])
        sc.op("dve", lambda e: e.tensor_tensor(out=tln, in0=mean, in1=mean, op=ALU.mult), reads=[b_mean], writes=[b_tln])
        sc.op("dve", lambda e: e.scalar_tensor_tensor(out=rstd, in0=ps[:, EB + 384:EB + 1536], scalar=1.0 / 2048, in1=tln,
                                                      op0=ALU.mult, op1=ALU.subtract), reads=[b_EB, b_tln], writes=[b_rstd])
        sc.op("dve", lambda e: e.tensor_scalar(out=rstd, in0=rstd, scalar1=1e-5, scalar2=None, op0=ALU.add),
              reads=[b_rstd], writes=[b_rstd])
        sc.op("act", lambda e: e.activation(out=rstd, in_=rstd, func=AF.Sqrt), reads=[b_rstd], writes=[b_rstd])
        sc.op("dve", lambda e: e.reciprocal(out=rstd, in_=rstd), reads=[b_rstd], writes=[b_rstd])
        for cc in range(16):
            sc.op("dve", lambda e, cc=cc: e.tensor_tensor(out=tln, in0=ySB[:, cc, :], in1=mean, op=ALU.subtract),
                  reads=[b_ySB, b_mean], writes=[b_tln])
            sc.op("dve", lambda e: e.tensor_tensor(out=tln, in0=tln, in1=rstd, op=ALU.mult), reads=[b_tln, b_rstd], writes=[b_tln])
            sc.op("act", lambda e, cc=cc: e.activation(out=zT[:, cc, :], in_=tln, func=AF.Silu, scale=lng[:, cc:cc + 1], bias=lnb[:, cc:cc + 1]),
                  reads=[b_tln, b_prm], writes=[b_zT])
        sc.barrier()

        sc.enabled = (upto >= 5)
        oTa = carve(0, (16, EXT), BF16)
        b_oTa = B("oTa")
        sc.op("sp", lambda e: e.dma_start(out=oTa, in_=oT_d.rearrange("h p t -> p h t")), reads=[B("oT")], writes=[b_oTa], dma=True)
        wpa = [carve(36 * KB, (16, 256), BF16), carve(44 * KB, (16, 256), BF16)]
        wpc = [carve(52 * KB, (16, 256), BF16), carve(60 * KB, (16, 256), BF16)]
        b_wpa, b_wpc = [B("wpa0"), B("wpa1")], [B("wpc0"), B("wpc1")]
        sga = [carve(108 * KB, (EXT,), BF16), carve(111 * KB, (EXT,), BF16)]
        sgc = [carve(114 * KB, (EXT,), BF16), carve(117 * KB, (EXT,), BF16)]
        b_sga, b_sgc = [B("sga0"), B("sga1")], [B("sgc0"), B("sgc1")]
        t1 = carve(120 * KB, (EXT,), F32)
        t2 = carve(125 * KB, (EXT,), F32)
        b_t1, b_t2 = B("t1"), B("t2")
        mst = [carve(130 * KB, (EXT,), BF16), carve(133 * KB, (EXT,), BF16)]
        b_mst = [B("mst0"), B("mst1")]
        for db in range(16):
            s_ = db % 2
            wload(wpa[s_], w_pa_v[:, :, db * 256:(db + 1) * 256], b_wpa[s_])
            wload(wpc[s_], w_pc_v[:, :, db * 256:(db + 1) * 256], b_wpc[s_])
            for c2 in range(2):
                dc = db * 2 + c2
                ms = dc % 2
                sc.op("sp", lambda e, ms=ms, dc=dc: e.dma_start(out=sga[ms], in_=sgT_d[dc]), reads=[B("sgT%d" % dc)], writes=[b_sga[ms]], dma=True)
                sc.op("sp", lambda e, ms=ms, dc=dc: e.dma_start(out=sgc[ms], in_=sgT_d[32 + dc]), reads=[B("sgT%d" % (32 + dc))], writes=[b_sgc[ms]], dma=True)
                sc.op("pe", lambda e, s_=s_, c2=c2: mm_ext(e, EA, lambda kc: wpa[s_][:, kc, c2 * 128:(c2 + 1) * 128], oTa, 16),
                      reads=[b_wpa[s_], b_oTa], writes=[b_EA])
                sc.op("pe", lambda e, s_=s_, c2=c2: mm_ext(e, EB, lambda kc: wpc[s_][:, kc, c2 * 128:(c2 + 1) * 128], zT, 16),
                      reads=[b_wpc[s_], b_zT], writes=[b_EB])
                sc.op("dve", lambda e, ms=ms: e.tensor_tensor(out=t1, in0=ps[:, EA + 384:EA + 1536], in1=sga[ms], op=ALU.mult),
                      reads=[b_EA, b_sga[ms]], writes=[b_t1])
                sc.op("dve", lambda e, ms=ms: e.tensor_tensor(out=t2, in0=ps[:, EB + 384:EB + 1536], in1=sgc[ms], op=ALU.mult),
                      reads=[b_EB, b_sgc[ms]], writes=[b_t2])
                sc.op("dve", lambda e, ms=ms: e.tensor_tensor(out=mst[ms], in0=t1, in1=t2, op=ALU.add),
                      reads=[b_t1, b_t2], writes=[b_mst[ms]])
                sc.op("sp", lambda e, ms=ms, dc=dc: e.dma_start(out=mT_d[dc], in_=mst[ms]), reads=[b_mst[ms]], writes=[B("mT")], dma=True)
        sc.barrier()

        sc.enabled = (upto >= 6)
        mTs = carve(0, (32, EXT), BF16)
        b_mTs = B("mTs")
        sc.op("sp", lambda e: e.dma_start(out=mTs, in_=mT_d.rearrange("c p t -> p c t")), reads=[B("mT")], writes=[b_mTs], dma=True)
        wo = [carve((72 + 16 * i) * KB, (32, 256), BF16) for i in range(3)]
        b_wo = [B("wo%d" % i) for i in range(3)]
        xe = [carve(120 * KB, (9, 128), F32), carve(125 * KB, (9, 128), F32)]
        b_xe = [B("xe0"), B("xe1")]
        hst = [carve(130 * KB, (EXT,), F32), carve(135 * KB, (EXT,), F32)]
        b_hst = [B("hst0"), B("hst1")]
        sq = carve(140 * KB, (EXT,), F32)
        b_sq = B("sq")
        rstdf = carve(145 * KB, (EXT,), F32)
        b_rstdf = B("rstdf")
        for ob in range(16):
            s_ = ob % 3
            wload(wo[s_], w_out_v[:, :, ob * 256:(ob + 1) * 256], b_wo[s_])
            for c2 in range(2):
                dc = ob * 2 + c2
                xs_ = dc % 2
                src = xr[E0:S, dc * 128:(dc + 1) * 128].rearrange("(i p) d -> p i d", p=128)
                sc.op("sp", lambda e, xs_=xs_, src=src: e.dma_start(out=xe[xs_], in_=src), writes=[b_xe[xs_]], dma=True)

                def omm(e, s_=s_, c2=c2, xs_=xs_):
                    for kc in range(32):
                        lw = wo[s_][:, kc, c2 * 128:(c2 + 1) * 128]
                        for (t0, n) in RNG:
                            e.matmul(ps[:, EA + 384 + t0: EA + 384 + t0 + n], lhsT=lw, rhs=mTs[:, kc, t0:t0 + n],
                                     start=(kc == 0), stop=False)
                    last = None
                    for i in range(9):
                        last = e.matmul(ps[:, EA + 384 + i * 128: EA + 384 + (i + 1) * 128], lhsT=xe[xs_][:, i, :], rhs=identf,
                                        start=False, stop=True, skip_group_check=True)
                    return last
                sc.op("pe", omm, reads=[b_wo[s_], b_mTs, b_xe[xs_], b_cf], writes=[b_EA])
                sc.op("act", lambda e, xs_=xs_: e.activation(out=hst[xs_], in_=ps[:, EA + 384:EA + 1536], func=AF.Copy),
                      reads=[b_EA], writes=[b_hst[xs_]])
                sc.op("dve", lambda e, xs_=xs_: e.tensor_tensor(out=sq, in0=hst[xs_], in1=hst[xs_], op=ALU.mult),
                      reads=[b_hst[xs_]], writes=[b_sq])
                sc.op("pe", lambda e, dc=dc: ones_mm(e, EB, sq, dc == 0, dc == 31), reads=[b_sq, b_cf], writes=[b_EB])
                sc.op("sp", lambda e, xs_=xs_, dc=dc: e.dma_start(out=hT_d[dc], in_=hst[xs_]), reads=[b_hst[xs_]], writes=[B("hT%d" % dc)], dma=True)
        sc.op("dve", lambda e: e.tensor_scalar(out=rstdf, in0=ps[:, EB + 384:EB + 1536], scalar1=1.0 / D, scalar2=1e-6, op0=ALU.mult, op1=ALU.add),
              reads=[b_EB], writes=[b_rstdf])
        sc.op("act", lambda e: e.activation(out=rstdf, in_=rstdf, func=AF.Sqrt), reads=[b_rstdf], writes=[b_rstdf])
        sc.op("dve", lambda e: e.reciprocal(out=rstdf, in_=rstdf), reads=[b_rstdf], writes=[b_rstdf])
        sc.barrier()

        sc.enabled = (upto >= 7)
        fT = carve(0, (32, EXT), BF16)
        b_fT = B("fT")
        hl = [carve(72 * KB, (EXT,), F32), carve(77 * KB, (EXT,), F32)]
        b_hl = [B("hl0"), B("hl1")]
        for dc in range(32):
            s_ = dc % 2
            sc.op("sp", lambda e, s_=s_, dc=dc: e.dma_start(out=hl[s_], in_=hT_d[dc]), reads=[B("hT%d" % dc)], writes=[b_hl[s_]], dma=True)
            sc.op("dve", lambda e, s_=s_, dc=dc: e.scalar_tensor_tensor(out=fT[:, dc, :], in0=hl[s_], scalar=gffn[:, dc:dc + 1], in1=rstdf,
                                                                    op0=ALU.mult, op1=ALU.mult),
                  reads=[b_hl[s_], b_rstdf, b_prm], writes=[b_fT])
        sc.op("dve", lambda e: e.tensor_scalar(out=fT[:, :, 0:128], in0=fT[:, :, 0:128], scalar1=flag, scalar2=None, op0=ALU.mult),
              reads=[b_fT, b_prm], writes=[b_fT])
        sc.barrier()

        sc.enabled = (upto >= 8)
        wg = [carve(72 * KB, (32, 256), BF16), carve(88 * KB, (32, 256), BF16)]
        wv = [carve(104 * KB, (32, 256), BF16), carve(120 * KB, (32, 256), BF16)]
        b_wg, b_wv = [B("wg0"), B("wg1")], [B("wv0"), B("wv1")]
        tg = carve(136 * KB, (TOK,), F32)
        tv = carve(140 * KB, (TOK,), F32)
        sgl = carve(144 * KB, (TOK,), F32)
        b_tg, b_tv, b_sgl = B("tg"), B("tv"), B("sgl")
        gst = [carve(148 * KB, (TOK,), BF16), carve(150 * KB, (TOK,), BF16)]
        b_gst = [B("gst0"), B("gst1")]
        O0 = 384 + 128

        def conv3(base, b_acc, dst, b_dst, ch):
            sc.op("act", lambda e: e.activation(out=dst, in_=ps[:, base + O0: base + O0 + TOK], func=AF.Identity,
                                                scale=wffn[:, ch, 2:3], bias=bffn[:, ch:ch + 1]),
                  reads=[b_acc, b_prm], writes=[b_dst])
            sc.op("dve", lambda e: e.scalar_tensor_tensor(out=dst, in0=ps[:, base + O0 - 1: base + O0 - 1 + TOK], scalar=wffn[:, ch, 1:2], in1=dst,
                                                          op0=ALU.mult, op1=ALU.add), reads=[b_acc, b_dst, b_prm], writes=[b_dst])
            sc.op("dve", lambda e: e.scalar_tensor_tensor(out=dst, in0=ps[:, base + O0 - 2: base + O0 - 2 + TOK], scalar=wffn[:, ch, 0:1], in1=dst,
                                                          op0=ALU.mult, op1=ALU.add), reads=[b_acc, b_dst, b_prm], writes=[b_dst])
        for fb in range(43):
            s_ = fb % 2
            wload(wg[s_], w_up_v[:, :, fb * 256:(fb + 1) * 256], b_wg[s_])
            wload(wv[s_], w_up_v[:, :, DFF + fb * 256: DFF + (fb + 1) * 256], b_wv[s_])
            for c2 in range(2):
                fc = fb * 2 + c2
                gs = fc % 2
                sc.op("pe", lambda e, s_=s_, c2=c2: mm_ext(e, EA, lambda kc: wg[s_][:, kc, c2 * 128:(c2 + 1) * 128], fT, 32),
                      reads=[b_wg[s_], b_fT], writes=[b_EA])
                sc.op("pe", lambda e, s_=s_, c2=c2: mm_ext(e, EB, lambda kc: wv[s_][:, kc, c2 * 128:(c2 + 1) * 128], fT, 32),
                      reads=[b_wv[s_], b_fT], writes=[b_EB])
                conv3(EA, b_EA, tg, b_tg, fc)
                conv3(EB, b_EB, tv, b_tv, NFC + fc)
                sc.op("act", lambda e: e.activation(out=sgl, in_=tg, func=AF.Silu), reads=[b_tg], writes=[b_sgl])
                sc.op("dve", lambda e, gs=gs: e.tensor_tensor(out=gst[gs], in0=sgl, in1=tv, op=ALU.mult),
                      reads=[b_sgl, b_tv], writes=[b_gst[gs]])
                sc.op("sp", lambda e, gs=gs, fc=fc: e.dma_start(out=gT_d[fc], in_=gst[gs]), reads=[b_gst[gs]], writes=[B("gT")], dma=True)
        sc.barrier()

        sc.enabled = (upto >= 9)
        HF = 43
        gTh = carve(0, (HF, TOK), BF16)
        b_gTh = B("gTh")
        wd = [carve((86 + 22 * i) * KB, (HF, 256), BF16) for i in range(3)]
        b_wd = [B("wd%d" % i) for i in range(3)]
        hres = [carve(152 * KB, (TOK,), F32), carve(156 * KB, (TOK,), F32)]
        b_hres = [B("hres0"), B("hres1")]
        h2st = [carve(160 * KB, (TOK,), F32), carve(164 * KB, (TOK,), F32)]
        b_h2st = [B("h2st0"), B("h2st1")]
        sq6 = carve(168 * KB, (TOK,), F32)
        b_sq6 = B("sq6")
        rstd2 = carve(172 * KB, (TOK,), F32)
        b_rstd2 = B("rstd2")
        hl6 = [carve(176 * KB, (512,), F32), carve(178 * KB, (512,), F32)]
        b_hl6 = [B("hl60"), B("hl61")]
        of6 = [carve(180 * KB, (512,), F32), carve(182 * KB, (512,), F32)]
        b_of6 = [B("of60"), B("of61")]
        ost = carve(0, (4, D), F32)
        b_ost = B("ost")
        dacc = [(0, [B("psbank0"), B("psbank1")]), (1024, [B("psbank2"), B("psbank3")])]
        SSB = 2048
        b_ss6 = [B("psbank4"), B("psbank5")]
        trb = [(2048, B("psbank4")), (2560, B("psbank5")), (3072, B("psbank6")), (3584, B("psbank7"))]
        trcnt = 0
        wcnt6 = 0
        for hf in range(2):
            f0 = hf * HF
            sc.op("sp", lambda e, f0=f0: e.dma_start(out=gTh, in_=gT_d[f0:f0 + HF, :, :].rearrange("c p t -> p c t")),
                  reads=[B("gT")], writes=[b_gTh], dma=True)
            for dc in range(32):
                s_ = dc % 2
                wc_ = dc % 2
                if wc_ == 0:
                    ws_ = wcnt6 % 3
                    wcnt6 += 1
                    wload(wd[ws_], w_down_v[:, f0:f0 + HF, (dc // 2) * 256:(dc // 2 + 1) * 256], b_wd[ws_])
                base, b_acc = dacc[dc % 2]
                if hf == 0:
                    sc.op("sp", lambda e, s_=s_, dc=dc: e.dma_start(out=hres[s_], in_=hT_d[dc, :, 128:EXT]),
                          reads=[B("hT%d" % dc)], writes=[b_hres[s_]], dma=True)
                else:
                    sc.op("sp", lambda e, s_=s_, dc=dc: e.dma_start(out=hres[s_], in_=h2T_d[dc]),
                          reads=[B("h2T%d" % dc)], writes=[b_hres[s_]], dma=True)

                def dmm6(e, ws_=ws_, wc_=wc_, base=base):
                    last = None
                    for fc in range(HF):
                        lw = wd[ws_][:, fc, wc_ * 128:(wc_ + 1) * 128]
                        for th in range(2):
                            last = e.matmul(ps[:, base + th * 512: base + (th + 1) * 512], lhsT=lw, rhs=gTh[:, fc, th * 512:(th + 1) * 512],
                                            start=(fc == 0), stop=(fc == HF - 1))
                    return last
                sc.op("pe", dmm6, reads=[b_wd[ws_], b_gTh], writes=b_acc)
                sc.op("dve", lambda e, s_=s_, base=base: e.tensor_tensor(out=h2st[s_], in0=ps[:, base:base + TOK], in1=hres[s_], op=ALU.add),
                      reads=b_acc + [b_hres[s_]], writes=[b_h2st[s_]])
                if hf == 1:
                    sc.op("act", lambda e, s_=s_: e.activation(out=sq6, in_=h2st[s_], func=AF.Square), reads=[b_h2st[s_]], writes=[b_sq6])

                    def ssmm(e, dc=dc):
                        last = None
                        for th in range(2):
                            last = e.matmul(ps[:, SSB + th * 512: SSB + (th + 1) * 512], lhsT=onesf, rhs=sq6[:, th * 512:(th + 1) * 512],
                                            start=(dc == 0), stop=(dc == 31))
                        return last
                    sc.op("pe", ssmm, reads=[b_sq6, b_cf], writes=b_ss6)
                sc.op("sp", lambda e, s_=s_, dc=dc: e.dma_start(out=h2T_d[dc], in_=h2st[s_]),
                      reads=[b_h2st[s_]], writes=[B("h2T%d" % dc)], dma=True)
            sc.barrier()
        sc.op("dve", lambda e: e.tensor_scalar(out=rstd2, in0=ps[:, SSB:SSB + TOK], scalar1=1.0 / D, scalar2=1e-6, op0=ALU.mult, op1=ALU.add),
              reads=b_ss6, writes=[b_rstd2])
        sc.op("act", lambda e: e.activation(out=rstd2, in_=rstd2, func=AF.Sqrt), reads=[b_rstd2], writes=[b_rstd2])
        sc.op("dve", lambda e: e.reciprocal(out=rstd2, in_=rstd2), reads=[b_rstd2], writes=[b_rstd2])
        sc.barrier()
        for half in range(2):
            t0h = half * 512
            for dc in range(32):
                s_ = dc % 2
                sc.op("sp", lambda e, s_=s_, dc=dc, t0h=t0h: e.dma_start(out=hl6[s_], in_=h2T_d[dc, :, t0h:t0h + 512]),
                      reads=[B("h2T%d" % dc)], writes=[b_hl6[s_]], dma=True)
                sc.op("dve", lambda e, s_=s_, dc=dc, t0h=t0h: e.scalar_tensor_tensor(out=of6[s_], in0=hl6[s_], scalar=gfin[:, dc:dc + 1],
                                                                                 in1=rstd2[:, t0h:t0h + 512], op0=ALU.mult, op1=ALU.mult),
                      reads=[b_hl6[s_], b_rstd2, b_prm], writes=[b_of6[s_]])
                for t in range(4):
                    tb, b_tb = trb[trcnt % 4]
                    trcnt += 1
                    sc.op("pe", lambda e, s_=s_, t=t, tb=tb: e.transpose(ps[:, tb:tb + 128], of6[s_][:, t * 128:(t + 1) * 128], identf),
                          reads=[b_of6[s_], b_cf], writes=[b_tb])
                    if t % 2 == 0:
                        sc.op("act", lambda e, t=t, tb=tb, dc=dc: e.activation(out=ost[:, t, dc * 128:(dc + 1) * 128], in_=ps[:, tb:tb + 128], func=AF.Copy),
                              reads=[b_tb], writes=[b_ost])
                    else:
                        sc.op("dve", lambda e, t=t, tb=tb, dc=dc: e.tensor_copy(out=ost[:, t, dc * 128:(dc + 1) * 128], in_=ps[:, tb:tb + 128]),
                              reads=[b_tb], writes=[b_ost])
            sc.op("sp", lambda e, t0h=t0h: e.dma_start(out=out_d[t0h:t0h + 512, :].rearrange("(t p) n -> p t n", p=128), in_=ost),
                  reads=[b_ost], writes=[B("out")], dma=True)
            sc.barrier()

        sc.finalize()
        sems = {}
        for e_ in ENGS:
            for i in range(NCS):
                sems[("c", e_, i)] = nc.alloc_semaphore("c_%s_%d" % (e_, i))
            if sc.ndma[e_] > 0:
                for i in range(NDS):
                    sems[("d", e_, i)] = nc.alloc_semaphore("d_%s_%d" % (e_, i))

        @block.sync
        def _(e):
            sc.emit("sp", e, sems, final_wait=True)

        @block.gpsimd
        def _(e):
            sc.emit("pool", e, sems)

        @block.tensor
        def _(e):
            sc.emit("pe", e, sems)

        @block.scalar
        def _(e):
            sc.emit("act", e, sems)

        @block.vector
        def _(e):
            sc.emit("dve", e, sems)
    return nc


def _host_consts(c):
    prm_common = None
    attc = np.zeros((128, NATT), np.float32)
    p = np.arange(128, dtype=np.float64)[:, None]
    for G in range(5):
        p0 = E0 if G == 0 else 7168 + 256 * (G - 1)
        kc = np.arange(64, dtype=np.float64)[None, :]
        attc[:, A_DK + G * 64: A_DK + (G + 1) * 64] = np.maximum(p0 - (kc * 128 + p), 0.0)
        n = np.arange(32)[None, :]
        ok = (n < 27 + G) & (n >= (7 - c) * 4)
        attc[:, A_GMASK + G * 32: A_GMASK + (G + 1) * 32] = np.where(ok, 0.0, -1e30)
        attc[:, A_GOK + G * 32: A_GOK + (G + 1) * 32] = np.where(ok, 1.0, 0.0)
    q = np.arange(256, dtype=np.float64)[None, :]
    for kj in range(2):
        dlt = q - (kj * 128 + p)
        attc[:, A_BTV + kj * 256: A_BTV + (kj + 1) * 256] = np.maximum(dlt, 0.0)
        attc[:, A_BTM + kj * 256: A_BTM + (kj + 1) * 256] = np.where(dlt >= 0, 0.0, NEG)
    attc[:, A_QO] = p[:, 0]
    attc[:, A_QO + 1] = p[:, 0] + 128
    kc = np.arange(64, dtype=np.float64)[None, :]
    attc[:, A_DK:A_DK + 64] = E0 - (kc * 128 + p)
    for G in range(5):
        attc[:, A_OWN + G * 32 + 27 + G] = 1.0
    for ti in range(9):
        attc[:, A_QO9 + ti] = p[:, 0] + 128 * ti
    return attc


_NC_CACHE = {}


def kernel(x, g_mix, w_in, w_conv_dw, b_conv_dw, ln_conv_g, ln_conv_b, w_proj_attn, w_proj_conv, w_out,
           g_ffn, w_up, w_ffn_dw, b_ffn_dw, w_down, g_final):
    f32 = np.float32
    x2 = np.asarray(x, f32).reshape(S, D)

    def lay(v, nch):
        return np.ascontiguousarray(np.asarray(v, f32).reshape(nch, 128).T)

    prm = np.zeros((128, NPRM), f32)
    prm[:, P_WCONV:P_WCONV + 496] = np.asarray(w_conv_dw, f32).reshape(31, 16, 128).transpose(2, 1, 0).reshape(128, 496)
    prm[:, P_BCONV:P_BCONV + 16] = lay(b_conv_dw, 16)
    prm[:, P_LNG:P_LNG + 16] = lay(ln_conv_g, 16)
    prm[:, P_LNB:P_LNB + 16] = lay(ln_conv_b, 16)
    prm[:, P_GFFN:P_GFFN + 32] = lay(g_ffn, 32)
    prm[:, P_GFIN:P_GFIN + 32] = lay(g_final, 32)
    prm[:, P_WFFN:P_WFFN + 516] = np.asarray(w_ffn_dw, f32).reshape(3, 172, 128).transpose(2, 1, 0).reshape(128, 516)
    prm[:, P_BFFN:P_BFFN + 172] = lay(b_ffn_dw, 172)
    gbc = np.ascontiguousarray(np.broadcast_to(np.asarray(g_mix, f32)[None, :], (128, D)))
    cf32 = np.concatenate([np.eye(128, dtype=f32), np.ones((128, 128), f32)], axis=1)
    identb = np.eye(128, dtype=f32).astype(ml_dtypes.bfloat16)
    oh2 = np.zeros((128, 32, 128), f32)
    for k in range(64):
        oh2[k, k % 32, :] = 1.0
    oh2 = oh2.reshape(128, 4096).astype(ml_dtypes.bfloat16)
    shared = {
        "w_in": np.ascontiguousarray(w_in, dtype=f32), "w_proj_attn": np.ascontiguousarray(w_proj_attn, dtype=f32),
        "w_proj_conv": np.ascontiguousarray(w_proj_conv, dtype=f32), "w_out": np.ascontiguousarray(w_out, dtype=f32),
        "w_up": np.ascontiguousarray(w_up, dtype=f32), "w_down": np.ascontiguousarray(w_down, dtype=f32),
        "gbc": gbc, "cf32": cf32, "identb": identb, "oh2": oh2,
    }
    in_maps = []
    for c in range(NCORE):
        xr = np.zeros((S, D), f32)
        n = (c + 1) * TOK
        xr[S - n:, :] = x2[:n, :]
        pc = prm.copy()
        pc[:, P_FLAG] = 0.0 if c == 0 else 1.0
        m = dict(shared)
        m["xr"] = xr
        m["prm"] = pc
        m["attc"] = _host_consts(c)
        in_maps.append(m)
    if "nc" not in _NC_CACHE:
        _NC_CACHE["nc"] = build()
    nc = _NC_CACHE["nc"]
    res = run_bass_kernel_spmd(nc, in_maps, core_ids=list(range(NCORE)))
    outs = [np.asarray(res.results[c]["out"], f32) for c in range(NCORE)]
    return np.concatenate(outs, axis=0).reshape(1, S, D)
```
